# Optimizing a Trainium2 kernel written in Bass

```python
import math
import jax
import jax.numpy as jnp
from jax import lax
import numpy as np

D_MODEL = 1024
BATCH = 8
SEQ = 2048
DEPTH = 4

N_MIXERS = 2
N_SUBLAYERS = 3
ATTN_HEADS = 8
ATTN_HEAD_DIM = D_MODEL // (2 * ATTN_HEADS)
ATTN_V_DIM = 2 * ATTN_HEAD_DIM
Q_BLOCK = 128
NUM_BUCKETS = 32
MAX_DISTANCE = 128
HGRN_EXPAND = 128
HGRN_HEADS = D_MODEL // HGRN_EXPAND
HGRN_FORGET_DIM = HGRN_HEADS * HGRN_EXPAND
HGRN_HEAD_V = D_MODEL // HGRN_HEADS
HGRN_CHUNK = 64
D_FF = 2816
N_ATTN_LAYERS = (DEPTH + 1) // 2
N_HGRN_LAYERS = DEPTH // 2
EPS = 1e-6

kernel_name = 'hybrid_diffattn_hgrn2_macaron_adaln'


def rms_norm(h, gain):
    h32 = h.astype(jnp.float32)
    h32 = h32 * lax.rsqrt(jnp.mean(h32 * h32, axis=-1, keepdims=True) + EPS)
    return (h32 * gain.astype(jnp.float32)).astype(h.dtype)


def t5_causal_bucket(dist):
    max_exact = NUM_BUCKETS // 2
    d = jnp.maximum(dist, 0)
    d_f = jnp.maximum(d, 1).astype(jnp.float32)
    large = max_exact + (jnp.log(d_f / max_exact) / math.log(MAX_DISTANCE / max_exact)
                         * (NUM_BUCKETS - max_exact)).astype(jnp.int32)
    large = jnp.minimum(large, NUM_BUCKETS - 1)
    return jnp.where(d < max_exact, d, large)


def swiglu(h, w_in, w_down):
    a, b = jnp.split(h @ w_in, 2, axis=-1)
    return (jax.nn.silu(a) * b) @ w_down


def diff_attention(h, w_qkv, w_o, q_gain, k_gain, lam, subln_gain, rel_bias, layer_idx):
    B, S, _ = h.shape
    H, hd, dv = ATTN_HEADS, ATTN_HEAD_DIM, ATTN_V_DIM
    q, k, v = jnp.split(h @ w_qkv, 3, axis=-1)
    q = rms_norm(q.reshape(B, S, H, 2, hd), q_gain) * (hd ** -0.5)
    k = rms_norm(k.reshape(B, S, H, 2, hd), k_gain)
    v = v.reshape(B, S, H, dv)
    lambda_init = 0.8 - 0.6 * math.exp(-0.3 * layer_idx)
    lam32 = lam.astype(jnp.float32)
    lam_full = (jnp.exp(jnp.sum(lam32[0] * lam32[1])) - jnp.exp(jnp.sum(lam32[2] * lam32[3]))
                + lambda_init)
    kt = k.transpose(0, 2, 3, 1, 4)
    vt = v.transpose(0, 2, 1, 3)
    nb = S // Q_BLOCK
    qb = q.reshape(B, nb, Q_BLOCK, H, 2, hd).transpose(1, 0, 3, 4, 2, 5)
    k_pos = jnp.arange(S)

    def block(args):
        q_blk, blk = args
        q_pos = blk * Q_BLOCK + jnp.arange(Q_BLOCK)
        dist = q_pos[:, None] - k_pos[None, :]
        bias = rel_bias[t5_causal_bucket(dist)].astype(jnp.float32).transpose(2, 0, 1)
        s = jnp.einsum('bhcqd,bhckd->bhcqk', q_blk, kt).astype(jnp.float32) + bias[None, :, None]
        s = jnp.where(dist >= 0, s, -jnp.inf)
        p = jax.nn.softmax(s, axis=-1)
        a = p[:, :, 0] - lam_full * p[:, :, 1]
        return jnp.einsum('bhqk,bhkv->bhqv', a, vt)

    o = lax.map(block, (qb, jnp.arange(nb)))
    o = o.transpose(1, 0, 3, 2, 4).reshape(B, S, H, dv)
    o = rms_norm(o, subln_gain) * (1.0 - lambda_init)
    return o.reshape(B, S, H * dv).astype(h.dtype) @ w_o


def hgrn2(h, w_in, w_o, out_gain, lb):
    B, S, _ = h.shape
    H, dk, dv, C = HGRN_HEADS, HGRN_EXPAND, HGRN_HEAD_V, HGRN_CHUNK
    F = HGRN_FORGET_DIM
    q, f, i, g = jnp.split(h @ w_in, [F, 2 * F, 2 * F + D_MODEL], axis=-1)
    f32 = f.astype(jnp.float32)
    lb32 = lb.astype(jnp.float32)
    log_forget = jnp.logaddexp(jnp.log(lb32), jnp.log1p(-lb32) + jax.nn.log_sigmoid(f32))
    k = (1.0 - lb32) * jax.nn.sigmoid(-f32)
    nc = S // C

    def to_chunks(t, d):
        return t.astype(jnp.float32).reshape(B, nc, C, H, d).transpose(1, 0, 3, 2, 4)

    qs, ks, gs = to_chunks(q, dk), to_chunks(k, dk), to_chunks(log_forget, dk)
    vs = to_chunks(i, dv)
    causal = jnp.tril(jnp.ones((C, C), dtype=bool))[None, None, :, :, None]

    def step(state, inp):
        qc, kc, vc, gc = inp
        G = jnp.cumsum(gc, axis=2)
        o_inter = jnp.einsum('bhtk,bhkv->bhtv', qc * jnp.exp(G), state)
        diff = G[:, :, :, None, :] - G[:, :, None, :, :]
        decay = jnp.exp(jnp.where(causal, diff, -jnp.inf))
        scores = jnp.einsum('bhtk,bhsk,bhtsk->bhts', qc, kc, decay)
        o_intra = jnp.einsum('bhts,bhsv->bhtv', scores, vc)
        G_last = G[:, :, -1:, :]
        new_state = (jnp.exp(G_last[:, :, 0, :])[..., None] * state
                     + jnp.einsum('bhsk,bhsv->bhkv', kc * jnp.exp(G_last - G), vc))
        return new_state, o_inter + o_intra

    state0 = jnp.zeros((B, H, dk, dv), jnp.float32)
    _, o = lax.scan(step, state0, (qs, ks, vs, gs))
    o = o.transpose(1, 0, 3, 2, 4).reshape(B, S, H, dv)
    o = rms_norm(o, out_gain.reshape(H, dv)) * jax.nn.silu(g.astype(jnp.float32)).reshape(B, S, H, dv)
    return o.reshape(B, S, D_MODEL).astype(h.dtype) @ w_o


def _normal(key, shape, scale):
    return jax.random.normal(key, shape, jnp.float32) * scale


def setup_inputs(seed: int = 0) -> dict:
    key = jax.random.key(seed)
    ks = jax.random.split(key, 20)
    D, F2 = D_MODEL, 2 * HGRN_FORGET_DIM + 2 * D_MODEL
    return {
        'x': _normal(ks[0], (BATCH, SEQ, D), 1.0),
        'c': _normal(ks[1], (BATCH, D), 1.0),
        'ada_w': _normal(ks[2], (DEPTH, D, N_SUBLAYERS * 3 * D), 0.5 * D ** -0.5),
        'ada_b': _normal(ks[3], (DEPTH, N_SUBLAYERS * 3 * D), 0.02),
        'norm_g': 1.0 + _normal(ks[4], (DEPTH, N_SUBLAYERS, D), 0.02),
        'ffn_w_in': _normal(ks[5], (DEPTH, 2, D, 2 * D_FF), D ** -0.5),
        'ffn_w_down': _normal(ks[6], (DEPTH, 2, D_FF, D), D_FF ** -0.5),
        'attn_w_qkv': _normal(ks[7], (N_ATTN_LAYERS, D, 3 * D), D ** -0.5),
        'attn_w_o': _normal(ks[8], (N_ATTN_LAYERS, D, D), D ** -0.5),
        'attn_q_gain': 1.0 + _normal(ks[9], (N_ATTN_LAYERS, ATTN_HEAD_DIM), 0.02),
        'attn_k_gain': 1.0 + _normal(ks[10], (N_ATTN_LAYERS, ATTN_HEAD_DIM), 0.02),
        'attn_lambda': _normal(ks[11], (N_ATTN_LAYERS, 4, ATTN_HEAD_DIM), 0.1),
        'attn_subln_gain': 1.0 + _normal(ks[12], (N_ATTN_LAYERS, ATTN_V_DIM), 0.02),
        'rel_bias': _normal(ks[13], (NUM_BUCKETS, ATTN_HEADS), 0.5),
        'hgrn_w_in': _normal(ks[14], (N_HGRN_LAYERS, D, F2), D ** -0.5),
        'hgrn_w_o': _normal(ks[15], (N_HGRN_LAYERS, D, D), D ** -0.5),
        'hgrn_out_gain': 1.0 + _normal(ks[16], (N_HGRN_LAYERS, D), 0.02),
        'hgrn_lb_logits': _normal(ks[17], (N_HGRN_LAYERS, HGRN_FORGET_DIM), 0.1),
    }


def reference(x, c, ada_w, ada_b, norm_g, ffn_w_in, ffn_w_down, attn_w_qkv, attn_w_o,
              attn_q_gain, attn_k_gain, attn_lambda, attn_subln_gain, rel_bias,
              hgrn_w_in, hgrn_w_o, hgrn_out_gain, hgrn_lb_logits):
    B = x.shape[0]
    lb_all = jnp.cumsum(jax.nn.softmax(hgrn_lb_logits.astype(jnp.float32), axis=0), axis=0)
    lb_all = lb_all - lb_all[0:1]
    c_act = jax.nn.silu(c)
    for layer in range(DEPTH):
        mod = (c_act @ ada_w[layer] + ada_b[layer]).reshape(B, N_SUBLAYERS, 3, D_MODEL)
        shift = mod[:, :, 0][:, None]
        scale = mod[:, :, 1][:, None]
        gate = mod[:, :, 2][:, None]
        h = rms_norm(x, norm_g[layer, 0]) * (1.0 + scale[:, :, 0]) + shift[:, :, 0]
        x = x + 0.5 * gate[:, :, 0] * swiglu(h, ffn_w_in[layer, 0], ffn_w_down[layer, 0])
        h = rms_norm(x, norm_g[layer, 1]) * (1.0 + scale[:, :, 1]) + shift[:, :, 1]
        j = layer // N_MIXERS
        if layer % N_MIXERS == 0:
            y = diff_attention(h, attn_w_qkv[j], attn_w_o[j], attn_q_gain[j], attn_k_gain[j],
                               attn_lambda[j], attn_subln_gain[j], rel_bias, layer)
        else:
            y = hgrn2(h, hgrn_w_in[j], hgrn_w_o[j], hgrn_out_gain[j], lb_all[j])
        x = x + gate[:, :, 1] * y
        h = rms_norm(x, norm_g[layer, 2]) * (1.0 + scale[:, :, 2]) + shift[:, :, 2]
        x = x + 0.5 * gate[:, :, 2] * swiglu(h, ffn_w_in[layer, 1], ffn_w_down[layer, 1])
    return x
```

```python
import math
import numpy as np
import concourse.bass as bass
import concourse.mybir as mybir
from concourse.bass_utils import run_bass_kernel_spmd

F32 = mybir.dt.float32
BF16 = mybir.dt.bfloat16
AF = mybir.ActivationFunctionType
ALU = mybir.AluOpType

DEPTH = 4
D = 1024
S = 2048
DFF = 2816
EPS = 1e-6
NB = 4
TB = 512
ENG = ['pe', 'act', 'dve', 'pool', 'sp']
BLKNAME = {'pe': 'tensor', 'act': 'scalar', 'dve': 'vector', 'pool': 'gpsimd', 'sp': 'sync'}
RING = 4
SLOT = 6144
NSC = 18


class St:
    __slots__ = ('w', 'r', 'rd')

    def __init__(self):
        self.w = None
        self.r = {}
        self.rd = []


class T:
    def __init__(self, ap, st=None):
        self.ap = ap
        self.st = st or St()

    def v(self, ap):
        return T(ap, self.st)

    def __getitem__(self, key):
        return T(self.ap[key], self.st)


class Op:
    __slots__ = ('eng', 'fn', 'deps', 'sig', 'sem', 'val', 'isdma')

    def __init__(self, eng, fn, dsem):
        self.eng = eng
        self.fn = fn
        self.deps = []
        self.sig = False
        self.sem = dsem
        self.val = 0
        self.isdma = dsem is not None


class Rec:
    def __init__(self, nc):
        self.nc = nc
        self.ops = {e: [] for e in ENG}
        self.all = []
        self.esem = {e: nc.alloc_semaphore("es_" + e) for e in ENG}
        self.dry = False
        self.nsem = 0

    def newsem(self):
        self.nsem += 1
        return self.nc.alloc_semaphore("ds_%d" % self.nsem)

    def op(self, eng, fn, reads=(), writes=(), dsem=None):
        if self.dry:
            return None
        o = Op(eng, fn, dsem)
        deps = []
        for t in reads:
            if t.st.w is not None:
                deps.append(t.st.w)
        for t in writes:
            if t.st.w is not None:
                deps.append(t.st.w)
            deps.extend(t.st.r.values())
            deps.extend(t.st.rd)
        seen = set()
        for d in deps:
            if d is o or id(d) in seen:
                continue
            seen.add(id(d))
            if d.eng == 'pe' and eng == 'pe' and not d.isdma and not o.isdma:
                continue
            o.deps.append(d)
        for t in writes:
            t.st.w = o
            t.st.r = {}
            t.st.rd = []
        for t in reads:
            if t.st.w is o:
                continue
            if o.isdma:
                t.st.rd.append(o)
            else:
                t.st.r[eng] = o
        self.ops[eng].append(o)
        self.all.append(o)
        return o

    def mm(self, out, lhsT, rhs, start=True, stop=True, extra_reads=()):
        return self.op('pe', lambda e: e.matmul(out.ap, lhsT.ap, rhs.ap, start=start, stop=stop),
                       [lhsT, rhs] + list(extra_reads), [out])

    def tr(self, out, in_, ident):
        return self.op('pe', lambda e: e.transpose(out.ap, in_.ap, ident.ap), [in_, ident], [out])

    def act(self, out, in_, func, bias=0.0, scale=1.0):
        reads = [in_]
        b = bias
        s = scale
        if isinstance(bias, T):
            reads.append(bias)
            b = bias.ap
        if isinstance(scale, T):
            reads.append(scale)
            s = scale.ap
        return self.op('act', lambda e: e.activation(out.ap, in_.ap, func, bias=b, scale=s), reads, [out])

    def tt(self, eng, out, in0, in1, op):
        return self.op(eng, lambda e: e.tensor_tensor(out.ap, in0.ap, in1.ap, op), [in0, in1], [out])

    def ts(self, eng, out, in0, s1, s2, op0, op1=None):
        reads = [in0]
        a1 = s1
        a2 = s2
        if isinstance(s1, T):
            reads.append(s1)
            a1 = s1.ap
        if isinstance(s2, T):
            reads.append(s2)
            a2 = s2.ap
        if op1 is None:
            return self.op(eng, lambda e: e.tensor_scalar(out.ap, in0.ap, a1, None, op0), reads, [out])
        return self.op(eng, lambda e: e.tensor_scalar(out.ap, in0.ap, a1, a2, op0, op1), reads, [out])

    def stt(self, out, in0, scalar, in1, op0, op1):
        reads = [in0, in1]
        sc = scalar
        if isinstance(scalar, T):
            reads.append(scalar)
            sc = scalar.ap
        return self.op('dve', lambda e: e.scalar_tensor_tensor(out.ap, in0.ap, sc, in1.ap, op0, op1), reads, [out])

    def copy(self, eng, out, in_):
        if eng == 'act':
            return self.op('act', lambda e: e.copy(out.ap, in_.ap), [in_], [out])
        return self.op(eng, lambda e: e.tensor_copy(out.ap, in_.ap), [in_], [out])

    def memset(self, eng, out, val):
        return self.op(eng, lambda e: e.memset(out.ap, val), [], [out])

    def dma(self, q, out, in_, sem, reads=(), writes=()):
        oa = out.ap if isinstance(out, T) else out
        ia = in_.ap if isinstance(in_, T) else in_
        rd = list(reads) + ([in_] if isinstance(in_, T) else [])
        wr = list(writes) + ([out] if isinstance(out, T) else [])
        return self.op(q, lambda e: e.dma_start(out=oa, in_=ia), rd, wr, dsem=sem)

    def waitfor(self, eng, ops):
        if self.dry:
            return
        o = Op(eng, None, None)
        o.deps = [d for d in ops if d is not None]
        self.ops[eng].append(o)
        self.all.append(o)

    def emit(self):
        nc = self.nc
        for o in self.all:
            for d in o.deps:
                d.sig = True
        cnt = {}
        for o in self.all:
            if o.isdma:
                k = id(o.sem)
                cnt[k] = cnt.get(k, 0) + 16
                o.val = cnt[k]
        for e in ENG:
            c = 0
            for o in self.ops[e]:
                if o.isdma or o.fn is None:
                    continue
                if o.sig:
                    c += 1
                    o.val = c
                    o.sem = self.esem[e]
        with nc.Block() as blk:
            for e in ENG:
                def body(engine, e=e):
                    waited = {}
                    for o in self.ops[e]:
                        for d in o.deps:
                            k = id(d.sem)
                            if waited.get(k, 0) < d.val:
                                engine.wait_ge(d.sem, d.val)
                                waited[k] = d.val
                        if o.fn is None:
                            continue
                        ins = o.fn(engine)
                        if o.isdma:
                            ins.then_inc(o.sem, 16)
                        elif o.sig:
                            ins.then_inc(o.sem, 1)
                getattr(blk, BLKNAME[e])(body)


def vec_layout():
    lay = {}
    pos = [0]

    def add(name, w):
        lay[name] = (pos[0], w)
        pos[0] += w
    add('c', 8)
    for l in range(DEPTH):
        add('adab%d' % l, 72)
        add('ng%d' % l, 24)
    for j in range(2):
        add('gq%d' % j, 2)
        add('gk%d' % j, 1)
        add('subg%d' % j, 1)
        add('lam%d' % j, 4)
        add('og%d' % j, 8)
        add('lbl%d' % j, 8)
    add('b31', 8)
    return lay, pos[0]


def t5_bucket(d):
    d = np.maximum(d, 0)
    df = np.maximum(d, 1).astype(np.float32)
    large = 16 + (np.log(df / 16) / math.log(8) * 16).astype(np.int32)
    large = np.minimum(large, 31)
    return np.where(d < 16, d, large)


def fm(v):
    return np.ascontiguousarray(v.reshape(-1, 128).T)


def build(layers, stop_after=None):
    nc = bass.Bass("TRN2", target_bir_lowering=False)
    R = Rec(nc)
    lay, NV = vec_layout()

    def din(name, shape, dt=F32):
        return nc.dram_tensor(name, list(shape), dt, kind="ExternalInput").ap()

    xin = din("xin", [D, S])
    outT = nc.dram_tensor("outT", [D, S], F32, kind="ExternalOutput").ap()
    ada_w = din("ada_w", [DEPTH, 12, 128, 3, 2048])
    w_in = din("ffn_w_in", [DEPTH, 2, 11, 128, 2, 2048])
    w_down = din("ffn_w_down", [DEPTH, 2, 11, 128, 2, 1024])
    w_qkv = din("attn_w_qkv", [2, 8, 128, 2, 1536])
    w_ao = din("attn_w_o", [2, D, D])
    w_hin = din("hgrn_w_in", [2, 8, 128, 2, 2048])
    w_ho = din("hgrn_w_o", [2, D, D])
    vecs_d = din("vecs", [128, NV])
    biasg_d = din("biasg", [8, 128, 256])
    cmask_d = din("cmask", [128, 256])
    reset_d = din("resetm", [128, 512], BF16)
    reset16_d = din("reset16", [128, 512], BF16)
    caus_d = din("caus64", [64, 64])
    ident_d = din("ident", [128, 128])

    def sb(name, shape, dt):
        return nc.alloc_sbuf_tensor(name, list(shape), dt)

    xT_t = sb("xT", [128, 8, S], F32)
    hT_t = sb("hT", [128, 8, S], BF16)
    X = [[T(xT_t[:, c, n * TB:(n + 1) * TB]) for n in range(NB)] for c in range(8)]
    HT = [[T(hT_t[:, c, n * TB:(n + 1) * TB]) for n in range(NB)] for c in range(8)]
    ring_t = [sb("ring%d" % i, [128, SLOT], BF16) for i in range(RING)]
    ringP = [[T(ring_t[i][:, p * 1024:(p + 1) * 1024]) for p in range(6)] for i in range(RING)]
    semP = [[R.newsem() for p in range(6)] for _ in range(RING)]

    def RW(s, view):
        return ringP[s][0].v(view), ringP[s][1:6]
    SC = [T(sb("sc%d" % i, [128, 512], F32)[:, :]) for i in range(NSC)]
    BG_t = [sb("bg%d" % i, [128, S], BF16) for i in range(4)]
    QN = [T(BG_t[0][:, n * TB:(n + 1) * TB]) for n in range(NB)]
    QN1 = [T(BG_t[3][:, n * TB:(n + 1) * TB]) for n in range(NB)]
    KN = [T(BG_t[1][:, n * TB:(n + 1) * TB]) for n in range(NB)]
    VT = [T(BG_t[2][:, n * TB:(n + 1) * TB]) for n in range(NB)]
    vecs = T(sb("vecs_s", [128, NV], F32)[:, :])
    resetm = T(sb("resetm_s", [128, 512], BF16)[:, :])
    reset16 = T(sb("reset16_s", [128, 512], BF16)[:, :])
    cmask = T(sb("cmask_s", [128, 256], F32)[:, :])
    caus = T(sb("caus_s", [64, 64], F32)[:, :])
    ident = T(sb("ident_s", [128, 128], F32)[:, :])
    ones_bf = T(sb("ones_bf", [128, 128], BF16)[:, :])
    ones_f = T(sb("ones_f", [128, 128], F32)[:, :])
    bd_bf = T(sb("bd_bf", [128, 128], BF16)[:, :])
    biasT = [T(sb("biasT%d" % i, [128, 256], F32)[:, :]) for i in range(1)]
    BTt = [T(sb("BT%d" % i, [128, 256], F32)[:, :]) for i in range(1)]
    modT2 = [T(sb("modT%d" % i, [128, 72], F32)[:, :]) for i in range(2)]
    gs2 = [T(sb("gs%d" % i, [128, 24], F32)[:, :]) for i in range(2)]
    gt2 = [T(sb("gt%d" % i, [128, 24], F32)[:, :]) for i in range(2)]
    cact = T(sb("cact", [128, 8], BF16)[:, :])
    lbv = T(sb("lbv", [128, 8], F32)[:, :])
    oml = T(sb("oml", [128, 8], F32)[:, :])
    neglam = T(sb("neglam", [128, 4], F32)[:, :])
    lamt = T(sb("lamt", [128, 4], F32)[:, :])
    Sst = T(sb("Sst", [128, 128], F32)[:, :])
    Sbf2 = [T(sb("Sbf%d" % i, [128, 128], BF16)[:, :]) for i in range(2)]
    PS = [T(nc.alloc_psum_tensor("ps%d" % i, [128, 512], F32)[:, :]) for i in range(8)]

    def V(name, lo=0, w=None):
        s0, ww = lay[name]
        if w is None:
            w = ww - lo
        return vecs[:, s0 + lo:s0 + lo + w]

    class WRing:
        def __init__(self):
            self.jobs = []
            self.pos = 0
            self.issued = 0

        def _issue(self, k):
            s = k % RING
            for p0, p1, src, dstfn in self.jobs[k]:
                R.dma('pool', ringP[s][p0].v(dstfn(ring_t[s])), src, semP[s][p0], writes=ringP[s][p0 + 1:p1])

        def next(self, spec):
            if R.dry:
                self.jobs.append(spec)
                return 0
            k = self.pos
            self.pos += 1
            while self.issued < min(k + RING - 1, len(self.jobs)):
                self._issue(self.issued)
                self.issued += 1
            return k % RING

    W = WRing()

    def setup():
        sx = [R.newsem() for _ in range(8)]
        for c in range(8):
            R.dma('sp', T(xT_t[:, c, :]), xin[c * 128:(c + 1) * 128, :], sx[c], writes=X[c])
        for dst, src in ((vecs, vecs_d), (resetm, reset_d), (reset16, reset16_d), (cmask, cmask_d), (caus, caus_d), (ident, ident_d)):
            R.dma('sp', dst, src, R.newsem())
        R.memset('dve', ones_bf, 1.0)
        R.memset('dve', ones_f, 1.0)
        R.memset('dve', bd_bf, 0.0)
        R.memset('dve', bd_bf[0:64, 0:64], 1.0)
        R.memset('dve', bd_bf[64:128, 64:128], 1.0)
        R.act(cact, V('c'), AF.Silu)

    def ada_tasks(l):
        modT, gs, gt = modT2[l % 2], gs2[l % 2], gt2[l % 2]
        tasks = []
        for q in range(12):
            def task(q=q):
                src = ada_w[l, q]
                s = W.next([(0, 6, src, lambda rt: rt[:, :].rearrange("p (a n) -> p a n", a=3))])
                if R.dry:
                    return
                wv = ring_t[s][:, :].rearrange("p (kc n) -> p kc n", kc=8)
                for cc in range(6):
                    col = q * 6 + cc
                    for kc in range(8):
                        lt, er = RW(s, wv[:, kc, cc * 128:(cc + 1) * 128])
                        R.mm(PS[7][:, col:col + 1], lt, cact[:, kc:kc + 1], start=(kc == 0), stop=(kc == 7),
                             extra_reads=er)
            tasks.append(task)

        def fin():
            R.tt('dve', modT, PS[7][:, 0:72], V('adab%d' % l), ALU.add)
            for s in range(3):
                R.stt(gs[:, s * 8:(s + 1) * 8], modT[:, s * 24 + 8:s * 24 + 16], 1.0, V('ng%d' % l, s * 8, 8),
                      ALU.add, ALU.mult)
                R.ts('dve', gt[:, s * 8:(s + 1) * 8], modT[:, s * 24 + 16:s * 24 + 24], 0.5 if s != 1 else 1.0, None,
                     ALU.mult)
        tasks.append(fin)
        return tasks

    def norm(l, s):
        modT, gs = modT2[l % 2], gs2[l % 2]
        sq_eng = ['dve', 'pool', 'dve', 'pool', 'dve', 'pool', 'dve', 'dve']
        for n in range(NB):
            for c in range(8):
                sq = SC[c % 2].v(SC[c % 2].ap.bitcast(BF16)[:, (n % 2) * 512:(n % 2 + 1) * 512])
                R.tt(sq_eng[c], sq, X[c][n], X[c][n], ALU.mult)
                R.mm(PS[6], ones_bf, sq, start=(c == 0), stop=(c == 7))
            lnv = SC[2]
            rstd = SC[3 + n % 2]
            R.act(lnv, PS[6], AF.Ln, bias=EPS, scale=1.0 / D)
            R.act(rstd, lnv, AF.Exp, scale=-0.5)
            for c in range(8):
                tmp = SC[5 + c % 2]
                R.tt('dve', tmp, X[c][n], rstd, ALU.mult)
                R.act(HT[c][n], tmp, AF.Identity, bias=modT[:, s * 24 + c:s * 24 + c + 1],
                      scale=gs[:, s * 8 + c:s * 8 + c + 1])

    def ffn(l, s, inject=None):
        gt = gt2[l % 2]
        inject = inject or []
        fi = 0 if s == 0 else 1
        pending = []
        step = 0
        for g in range(11):
            sl = W.next([(0, 4, w_in[l, fi, g], lambda rt: rt[:, 0:4096].rearrange("p (a n) -> p a n", a=2)),
                         (4, 6, w_down[l, fi, g], lambda rt: rt[:, 4096:SLOT].rearrange("p (m d) -> p m d", m=2))])
            if R.dry:
                if inject:
                    inject.pop(0)()
                continue
            wab = ring_t[sl][:, 0:4096].rearrange("p (kc ab f) -> p kc ab f", kc=8, ab=2)
            wa0 = wab[:, :, 0, :]
            wa1 = wab[:, :, 1, :]
            wb = ring_t[sl][:, 4096:SLOT].rearrange("p (m d) -> p m d", m=2)
            for n in range(NB):
                us = []
                for m in range(2):
                    pa = PS[(step % 2) * 2]
                    pb = PS[(step % 2) * 2 + 1]
                    for kc in range(8):
                        lt, er = RW(sl, wa0[:, kc, m * 128:(m + 1) * 128])
                        R.mm(pa, lt, HT[kc][n], start=(kc == 0), stop=(kc == 7), extra_reads=er)
                    for kc in range(8):
                        lt, er = RW(sl, wa1[:, kc, m * 128:(m + 1) * 128])
                        R.mm(pb, lt, HT[kc][n], start=(kc == 0), stop=(kc == 7), extra_reads=er)
                    silt = SC[7 + step % 2]
                    bt = SC[9 + step % 2]
                    R.act(silt, pa, AF.Silu)
                    R.copy('act', bt, pb)
                    ut = SC[11 + (step % 4)]
                    u = ut.v(ut.ap.bitcast(BF16)[:, 0:512])
                    R.tt('pool', u, silt, bt, ALU.mult)
                    us.append(u)
                    step += 1
                    if len(pending) > (1 if m == 0 else 0) and pending:
                        pending.pop(0)()

                def mk_down(j0, us=us, n=n, sl=sl, wb=wb):
                    def down():
                        for j in range(j0, j0 + 4):
                            py = PS[4 + j % 3]
                            for m in range(2):
                                lt, er = RW(sl, wb[:, m, j * 128:(j + 1) * 128])
                                R.mm(py, lt, us[m], start=(m == 0), stop=(m == 1), extra_reads=er)
                            R.stt(X[j][n], py, gt[:, s * 8 + j:s * 8 + j + 1], X[j][n], ALU.mult, ALU.add)
                    return down
                pending.append(mk_down(0))
                pending.append(mk_down(4))
            if inject:
                inject.pop(0)()
                while pending:
                    pending.pop(0)()
        while pending:
            pending.pop(0)()
        while inject:
            inject.pop(0)()

    def attn(l):
        gt = gt2[l % 2]
        j = l // 2
        lam_init = 0.8 - 0.6 * math.exp(-0.3 * l)
        R.tt('dve', lamt[:, 0:1], V('lam%d' % j, 0, 1), V('lam%d' % j, 1, 1), ALU.mult)
        R.tt('dve', lamt[:, 1:2], V('lam%d' % j, 2, 1), V('lam%d' % j, 3, 1), ALU.mult)
        R.mm(PS[7][:, 0:2], ones_f, lamt[:, 0:2])
        R.act(lamt[:, 2:4], PS[7][:, 0:2], AF.Exp)
        R.tt('dve', neglam[:, 0:1], lamt[:, 3:4], lamt[:, 2:3], ALU.subtract)
        R.ts('dve', neglam[:, 1:2], neglam[:, 0:1], -lam_init, None, ALU.add)
        NL = neglam[:, 1:2]
        st = {'tix': 0, 'pidx': 0}
        epi = []
        for h in range(8):
            srcB = w_ao[j][h * 128:(h + 1) * 128, :]
            spec = [(0, 3, w_qkv[j, h], lambda rt: rt[:, 0:3072].rearrange("p (a n) -> p a n", a=2)),
                    (3, 4, srcB, lambda rt: rt[:, 3072:4096])]
            sl = W.next(spec)
            if R.dry:
                continue
            wa = ring_t[sl][:, 0:3072].rearrange("p (kc t f) -> p kc t f", kc=8, t=3)
            wo = ring_t[sl][:, 3072:4096]
            bT = biasT[0]
            BT = BTt[0]
            R.dma('sp', bT, biasg_d[h], R.newsem())
            R.tt('pool', BT, bT, cmask, ALU.add)
            b31 = V('b31', h, 1)
            tails = []
            kidx = 0
            for which, dstl, gain, lnb in ((0, QN, V('gq%d' % j, 0, 1), math.log(0.125)), (1, KN, V('gk%d' % j), 0.0)):
                for n in range(NB):
                    pq = PS[kidx % 2]
                    for kc in range(8):
                        lt, er = RW(sl, wa[:, kc, which, :])
                        R.mm(pq, lt, HT[kc][n], start=(kc == 0), stop=(kc == 7), extra_reads=er)
                    qs = SC[kidx % 2]
                    R.copy('act', qs, pq)
                    sq = SC[2].v(SC[2].ap.bitcast(BF16)[:, (kidx % 2) * 512:(kidx % 2 + 1) * 512])
                    R.tt('dve', sq, qs, qs, ALU.mult)

                    def tail(which=which, dstl=dstl, gain=gain, lnb=lnb, n=n, qs=qs, sq=sq, k2=kidx % 2):
                        R.mm(PS[2 + k2], bd_bf, sq)
                        lnv = SC[3].v(SC[3].ap[:, :]) if k2 == 0 else SC[9].v(SC[9].ap[:, :])
                        rs = SC[4].v(SC[4].ap[:, :]) if k2 == 0 else SC[10].v(SC[10].ap[:, :])
                        R.act(lnv, PS[2 + k2], AF.Ln, bias=EPS, scale=1.0 / 64)
                        R.act(rs, lnv, AF.Exp, bias=lnb, scale=-0.5)
                        R.stt(dstl[n], qs, gain, rs, ALU.mult, ALU.mult)
                        if which == 0:
                            R.stt(QN1[n], qs, V('gq%d' % j, 1, 1), rs, ALU.mult, ALU.mult)
                    tails.append(tail)
                    if len(tails) > 1:
                        tails.pop(0)()
                    kidx += 1
                    if epi:
                        epi.pop(0)()
            for tt_ in range(16):
                pv = PS[4 + (tt_ // 4) % 2]
                for kc in range(8):
                    lt, er = RW(sl, wa[:, kc, 2, :])
                    R.mm(pv[:, (tt_ % 4) * 128:(tt_ % 4 + 1) * 128],
                         HT[kc][tt_ // 4].v(hT_t[:, kc, tt_ * 128:(tt_ + 1) * 128]),
                         lt, start=(kc == 0), stop=(kc == 7), extra_reads=er)
                if tt_ % 4 == 3:
                    R.copy('dve', VT[tt_ // 4], pv)
                    while tails:
                        tails.pop(0)()
            for jq in range(NB):
                nk = 4 * jq + 4
                pend = []
                nd = list(range(0, max(4 * jq - 1, 0)))
                dg = list(range(max(4 * jq - 1, 0), nk))
                order = []
                while nd or dg:
                    if nd:
                        order.append(nd.pop(0))
                    if dg and (order or not nd):
                        order.append(dg.pop(0))
                for oi, i in enumerate(order):
                    delta = 128 * i - 512 * jq
                    c0 = max(delta, 0)
                    diag = i >= 4 * jq - 1
                    for c in range(2):
                        ps = PS[st['tix'] % 3]
                        st['tix'] += 1
                        kt = KN[i // 4].v(BG_t[1][:, i * 128:(i + 1) * 128])
                        qsrc = (QN, QN1)[c][jq]
                        qt = qsrc.v(BG_t[0 if c == 0 else 3][:, jq * 512 + c0:(jq + 1) * 512])
                        R.mm(ps[:, c0:512], kt, qt)
                        pk = st['pidx'] % 6
                        ptile = SC[5 + pk % 3]
                        st['pidx'] += 1
                        pt = ptile.v(ptile.ap.bitcast(BF16)[:, (pk // 3) * 512:(pk // 3 + 1) * 512])
                        if not diag:
                            R.act(pt, ps, AF.Exp, bias=b31)
                        else:
                            cb1 = min(delta + 256, 512)
                            tmp = SC[9 + c]
                            R.tt('dve', tmp[:, c0:cb1], ps[:, c0:cb1], BT[:, c0 - delta:cb1 - delta], ALU.add)
                            R.act(pt[:, c0:cb1], tmp[:, c0:cb1], AF.Exp)
                            if cb1 < 512:
                                R.act(pt[:, cb1:512], ps[:, cb1:512], AF.Exp, bias=b31)

                        def av(i=i, c=c, pt=pt, c0=c0, first=(oi == 0), last=(oi == nk - 1)):
                            vt = VT[i // 4][:, (i % 4) * 128:(i % 4 + 1) * 128]
                            R.mm(PS[4 + c][:, c0:512], vt, pt[:, c0:512], start=first, stop=last)
                            R.mm(PS[6 + c][:, c0:512], ones_bf, pt[:, c0:512], start=first, stop=last)
                        pend.append(av)
                        if len(pend) > 2:
                            pend.pop(0)()
                        if epi:
                            epi.pop(0)()
                while pend:
                    pend.pop(0)()
                r0, r1, t0, t1, ob = SC[11], SC[12], SC[13], SC[14], SC[15]
                R.op('dve', lambda e, a=r0, b=PS[6]: e.reciprocal(a.ap, b.ap), [PS[6]], [r0])
                R.copy('act', t0, PS[4])
                R.op('dve', lambda e, a=r1, b=PS[7]: e.reciprocal(a.ap, b.ap), [PS[7]], [r1])
                R.copy('act', t1, PS[5])
                R.tt('dve', t0, t0, r0, ALU.mult)
                R.tt('pool', t1, t1, r1, ALU.mult)
                R.stt(ob, t1, NL, t0, ALU.mult, ALU.add)
                sq = SC[8].v(SC[8].ap.bitcast(BF16)[:, 0:512])
                R.tt('pool', sq, ob, ob, ALU.mult)
                oh = SC[8].v(SC[8].ap.bitcast(BF16)[:, 512:1024])

                def stage_b(sq=sq, ob=ob, oh=oh):
                    R.mm(PS[3], ones_bf, sq)
                    R.act(SC[11], PS[3], AF.Ln, bias=EPS, scale=1.0 / 128)
                    R.act(SC[12], SC[11], AF.Exp, bias=math.log(1.0 - lam_init), scale=-0.5)
                    R.stt(oh, ob, V('subg%d' % j), SC[12], ALU.mult, ALU.mult)
                epi.append(stage_b)
                for jj in range(8):
                    def stage_c(jj=jj, jq=jq, oh=oh, sl=sl, wo=wo):
                        lt, er = RW(sl, wo[:, jj * 128:(jj + 1) * 128])
                        R.mm(PS[3], lt, oh, extra_reads=er)
                        R.stt(X[jj][jq], PS[3], gt[:, 8 + jj:8 + jj + 1], X[jj][jq], ALU.mult, ALU.add)
                    epi.append(stage_c)
        while epi:
            epi.pop(0)()

    def hgrn(l):
        gt = gt2[l % 2]
        j = l // 2
        if j == 0:
            R.memset('dve', lbv, 0.0)
        else:
            R.tt('dve', lbv, V('lbl1'), V('lbl0'), ALU.subtract)
            R.act(lbv, lbv, AF.Sigmoid)
        R.ts('dve', oml, lbv, -1.0, 1.0, ALU.mult, ALU.add)
        sig, fg, lg, G, G16, eG, kk, kh, gsig, gsilu = SC[0:10]
        eL = gsig
        qq = SC[10].v(SC[10].ap.bitcast(BF16))
        Qs, qh = qq[:, 0:512], qq[:, 512:1024]
        KA = SC[11].v(SC[11].ap.bitcast(BF16))
        KB = SC[14].v(SC[14].ap.bitcast(BF16))
        Kp = [KA[:, 0:512], KA[:, 512:1024], KB[:, 0:512], KB[:, 512:1024]]
        vT = SC[12].v(SC[12].ap.bitcast(BF16).rearrange("p (c d) -> p c d", c=8)[0:64])
        khT = SC[13].v(SC[13].ap.bitcast(BF16).rearrange("p (c d) -> p c d", c=8)[0:64])
        BGH = [T(BG_t[k // 2][:, :].bitcast(F32)[:, (k % 2) * 512:(k % 2 + 1) * 512]) for k in range(8)]
        E = [SC[16], SC[17], BGH[5], BGH[6]]
        SbfAll = BGH[7].v(BGH[7].ap.bitcast(BF16).rearrange("p (c d) -> p c d", c=8))
        gsilu2 = [gsilu, BGH[0]]
        osb, lnv, tt_o = BGH[1], BGH[3], BGH[4]
        sqo = BGH[2].v(BGH[2].ap.bitcast(BF16))
        sq, oh = sqo[:, 0:512], sqo[:, 512:1024]
        outq = []
        blk = [0]

        def popq(k=1):
            for _ in range(k):
                if outq:
                    outq.pop(0)()
        for e_ in E:
            R.memset('pool', e_, 0.0)
        R.memset('dve', PS[7], 0.0)
        for h in range(8):
            srcB = w_ho[j][h * 128:(h + 1) * 128, :]
            spec = [(0, 4, w_hin[j, h], lambda rt: rt[:, 0:4096].rearrange("p (a n) -> p a n", a=2)),
                    (4, 5, srcB, lambda rt: rt[:, 4096:5120])]
            sl = W.next(spec)
            if R.dry:
                continue
            wa = ring_t[sl][:, 0:4096].rearrange("p (kc t f) -> p kc t f", kc=8, t=4)
            wo = ring_t[sl][:, 4096:5120]
            R.memset('dve', Sst, 0.0)
            R.memset('pool', SbfAll[:, 0, :], 0.0)
            og = V('og%d' % j, h, 1)
            for n in range(NB):
                gsl = gsilu2[blk[0] % 2]
                blk[0] += 1
                for kc in range(8):
                    lt, er = RW(sl, wa[:, kc, 1, :])
                    R.mm(PS[0], lt, HT[kc][n], start=(kc == 0), stop=(kc == 7), extra_reads=er)
                R.act(sig, PS[0], AF.Sigmoid)
                for kc in range(8):
                    lt, er = RW(sl, wa[:, kc, 3, :])
                    R.mm(PS[2], lt, HT[kc][n], start=(kc == 0), stop=(kc == 7), extra_reads=er)
                R.act(gsig, PS[2], AF.Sigmoid)
                R.tt('dve', gsl, PS[2], gsig, ALU.mult)
                popq()
                R.ts('dve', fg, sig, oml[:, h:h + 1], lbv[:, h:h + 1], ALU.mult, ALU.add)
                R.act(lg, fg, AF.Ln)
                R.ts('pool', kk, fg, -1.0, 1.0, ALU.mult, ALU.add)
                popq()
                R.op('dve', lambda e, o_=G, d1=lg: e.tensor_tensor_scan(o_.ap, resetm.ap, d1.ap, 0.0, ALU.mult, ALU.add),
                     [resetm, lg], [G])
                R.op('dve', lambda e, o_=G16, d1=lg: e.tensor_tensor_scan(o_.ap, reset16.ap, d1.ap, 0.0, ALU.mult, ALU.add),
                     [reset16, lg], [G16])
                R.act(eG, G, AF.Exp)
                R.act(G16, G16, AF.Exp)
                popq()
                Gv = G.ap.rearrange("p (c t) -> p c t", c=8)
                for i_ in range(4):
                    w_ = 16 * (i_ + 1)
                    Ev = E[i_].v(E[i_].ap.rearrange("p (c t) -> p c t", c=8)[:, :, 0:w_])
                    if i_ == 0:
                        R.act(Ev, G.v(Gv[:, :, 0:w_]), AF.Exp, scale=-1.0)
                    else:
                        R.tt('dve', Ev, G.v(Gv[:, :, 16 * i_ - 1:16 * i_].to_broadcast([128, 8, w_])),
                             G.v(Gv[:, :, 0:w_]), ALU.subtract)
                        R.act(Ev, Ev, AF.Exp)
                R.tt('dve', eL, G.v(Gv[:, :, 63:64].to_broadcast([128, 8, 64])), G.v(Gv), ALU.subtract)
                R.act(eL, eL, AF.Exp)
                popq()
                for kc in range(8):
                    lt, er = RW(sl, wa[:, kc, 0, :])
                    R.mm(PS[1], lt, HT[kc][n], start=(kc == 0), stop=(kc == 7), extra_reads=er)
                R.tt('dve', Qs, PS[1], G16, ALU.mult)
                R.tt('dve', qh, PS[1], eG, ALU.mult)
                for i_ in range(4):
                    R.tt('dve' if i_ < 2 else 'pool', Kp[i_], kk, E[i_], ALU.mult)
                R.tt('pool', kh, kk, eL, ALU.mult)
                popq()
                for ci in range(8):
                    pv = PS[3 + ci // 4]
                    for kc in range(8):
                        lt, er = RW(sl, wa[:, kc, 2, :])
                        R.mm(pv[0:64, (ci % 4) * 128:(ci % 4 + 1) * 128],
                             HT[kc][n].v(hT_t[:, kc, n * 512 + ci * 64:n * 512 + ci * 64 + 64]),
                             lt, start=(kc == 0), stop=(kc == 7), extra_reads=er)
                R.copy('act', vT[:, 0:4, :], PS[3].v(PS[3].ap[0:64, :].rearrange("p (c d) -> p c d", c=4)))
                R.copy('act', vT[:, 4:8, :], PS[4].v(PS[4].ap[0:64, :].rearrange("p (c d) -> p c d", c=4)))
                popq()
                for ci in range(8):
                    pt_ = PS[5 + ci // 4]
                    R.tr(pt_[0:64, (ci % 4) * 128:(ci % 4 + 1) * 128], kh[:, ci * 64:ci * 64 + 64], ident)
                R.copy('dve', khT[:, 0:4, :], PS[5].v(PS[5].ap[0:64, :].rearrange("p (c d) -> p c d", c=4)))
                R.copy('dve', khT[:, 4:8, :], PS[6].v(PS[6].ap[0:64, :].rearrange("p (c d) -> p c d", c=4)))
                popq()
                PSO = PS[2]
                AbfAll = SC[15].v(SC[15].ap.bitcast(BF16)[0:64, 512:1024])
                for ci in range(8):
                    cs = ci * 64
                    for i_ in range(4):
                        w_ = 16 * (i_ + 1)
                        R.mm(PS[7][0:w_, cs + 16 * i_:cs + 16 * i_ + 16], Kp[i_][:, cs:cs + w_],
                             Qs[:, cs + 16 * i_:cs + 16 * i_ + 16])
                R.tt('dve', AbfAll.v(AbfAll.ap.rearrange("p (c t) -> p c t", c=8)),
                     PS[7].v(PS[7].ap[0:64, :].rearrange("p (c t) -> p c t", c=8)),
                     caus.v(caus.ap.rearrange("p (o t) -> p o t", o=1).to_broadcast([64, 8, 64])), ALU.mult)
                popq()
                for ci in range(8):
                    pu = PS[ci // 4][:, (ci % 4) * 128:(ci % 4 + 1) * 128]
                    R.mm(pu, khT[:, ci, :], vT[:, ci, :])
                popq(2)
                def pe_chunk(ci):
                    cs = ci * 64
                    R.mm(PSO[:, cs:cs + 64], vT[:, ci, :], AbfAll[:, cs:cs + 64], start=True, stop=False)
                    R.mm(PSO[:, cs:cs + 64], SbfAll[:, ci, :], qh[:, cs:cs + 64], start=False, stop=True)
                pe_chunk(0)
                for ci in range(8):
                    cs = ci * 64
                    pu = PS[ci // 4][:, (ci % 4) * 128:(ci % 4 + 1) * 128]
                    R.stt(SbfAll[:, (ci + 1) % 8, :], Sst, eG[:, cs + 63:cs + 64], pu, ALU.mult, ALU.add)
                    R.stt(Sst, Sst, eG[:, cs + 63:cs + 64], pu, ALU.mult, ALU.add)
                for ci in range(1, 8):
                    pe_chunk(ci)
                R.copy('act', osb, PSO)

                def s0(gsl=gsl, og=og):
                    R.tt('pool', sq, osb, osb, ALU.mult)
                    R.mm(PS[6], ones_bf, sq)
                    R.act(lnv, PS[6], AF.Ln, bias=EPS, scale=1.0 / 128)
                    R.act(lnv, lnv, AF.Exp, scale=-0.5)
                    R.stt(tt_o, osb, og, lnv, ALU.mult, ALU.mult)
                    R.tt('pool', oh, tt_o, gsl, ALU.mult)
                outq.append(s0)
                for jj in range(8):
                    def sy(jj=jj, n=n, sl=sl, wo=wo):
                        lt, er = RW(sl, wo[:, jj * 128:(jj + 1) * 128])
                        R.mm(PS[6], lt, oh, extra_reads=er)
                        R.stt(X[jj][n], PS[6], gt[:, 8 + jj:8 + jj + 1], X[jj][n], ALU.mult, ALU.add)
                    outq.append(sy)
        popq(100)

    def finish():
        so = [R.newsem() for _ in range(8)]
        outs = []
        for c in range(8):
            outs.append(R.dma('sp', outT[c * 128:(c + 1) * 128, :], T(xT_t[:, c, :]), so[c], reads=X[c]))
        R.waitfor('sp', [o for o in R.all if o.isdma])

    def program():
        if not R.dry:
            setup()
        done = False
        for idx, l in enumerate(layers):
            if idx == 0:
                for t_ in ada_tasks(l):
                    t_()
            for s in range(3):
                if not R.dry:
                    norm(l, s)
                if s == 1:
                    if l % 2 == 0:
                        attn(l)
                    else:
                        hgrn(l)
                else:
                    inj = None
                    last = stop_after is not None and (l, s) == tuple(stop_after)
                    if s == 2 and idx + 1 < len(layers) and not last:
                        inj = ada_tasks(layers[idx + 1])
                    ffn(l, s, inj)
                if stop_after is not None and (l, s) == tuple(stop_after):
                    done = True
                    break
            if done:
                break
        if not R.dry:
            finish()

    R.dry = True
    program()
    R.dry = False
    program()
    R.emit()
    return nc


LAUNCH_GROUPS = [[0, 1, 2, 3]]
_NC_CACHE = {}


def _get_nc(layers, stop_after=None):
    key = (tuple(layers), stop_after)
    if key not in _NC_CACHE:
        _NC_CACHE[key] = build(list(layers), stop_after)
    return _NC_CACHE[key]


def host_consts(inp, b):
    lay, NV = vec_layout()
    vecs = np.zeros((128, NV), np.float32)

    def put(name, arr):
        s0, w = lay[name]
        vecs[:, s0:s0 + w] = arr.reshape(128, w)
    put('c', fm(inp['c'][b]))
    for l in range(DEPTH):
        put('adab%d' % l, fm(inp['ada_b'][l]))
        put('ng%d' % l, fm(inp['norm_g'][l].reshape(-1)))
    for j in range(2):
        gq = np.zeros((128, 2), np.float32)
        gq[0:64, 0] = inp['attn_q_gain'][j]
        gq[64:128, 1] = inp['attn_q_gain'][j]
        put('gq%d' % j, gq)
        put('gk%d' % j, np.tile(inp['attn_k_gain'][j], 2).reshape(128, 1))
        put('subg%d' % j, inp['attn_subln_gain'][j].reshape(128, 1))
        lam = np.zeros((128, 4), np.float32)
        lam[0:64, :] = inp['attn_lambda'][j].T
        put('lam%d' % j, lam)
        put('og%d' % j, fm(inp['hgrn_out_gain'][j]))
        put('lbl%d' % j, fm(inp['hgrn_lb_logits'][j]))
    put('b31', np.broadcast_to(inp['rel_bias'][31][None, :], (128, 8)).copy())
    return vecs


def shared_consts(inp):
    kk = np.arange(128)[:, None]
    e = np.arange(256)[None, :]
    dist = e - kk
    bidx = t5_bucket(dist)
    biasg = np.ascontiguousarray(np.transpose(inp['rel_bias'][bidx], (2, 0, 1))).astype(np.float32)
    cmask = np.where(dist >= 0, 0.0, -30000.0).astype(np.float32)
    resetm = np.ones((128, 512), np.float32)
    resetm[:, 0::64] = 0.0
    reset16 = np.ones((128, 512), np.float32)
    reset16[:, 0::16] = 0.0
    s_ = np.arange(64)[:, None]
    t_ = np.arange(64)[None, :]
    caus = (s_ <= t_).astype(np.float32)
    ident = np.eye(128, dtype=np.float32)
    import ml_dtypes
    return dict(biasg=biasg, cmask=cmask, resetm=resetm.astype(ml_dtypes.bfloat16), reset16=reset16.astype(ml_dtypes.bfloat16), caus64=caus, ident=ident)


WKEYS = ['ada_w', 'ffn_w_in', 'ffn_w_down', 'attn_w_qkv', 'attn_w_o', 'hgrn_w_in', 'hgrn_w_o']


def host_weights(inp):
    c = np.ascontiguousarray
    out = {}
    a = inp['ada_w'].reshape(DEPTH, 8, 128, 12, 768)
    out['ada_w'] = c(a.transpose(0, 3, 2, 1, 4)).reshape(DEPTH, 12, 128, 3, 2048)
    a = inp['ffn_w_in'].reshape(DEPTH, 2, 8, 128, 2, 11, 256)
    out['ffn_w_in'] = c(a.transpose(0, 1, 5, 3, 2, 4, 6)).reshape(DEPTH, 2, 11, 128, 2, 2048)
    a = inp['ffn_w_down'].reshape(DEPTH, 2, 11, 2, 128, 1024)
    out['ffn_w_down'] = c(a.transpose(0, 1, 2, 4, 3, 5))
    a = inp['attn_w_qkv'].reshape(2, 8, 128, 3, 8, 128)
    out['attn_w_qkv'] = c(a.transpose(0, 4, 2, 1, 3, 5)).reshape(2, 8, 128, 2, 1536)
    a = inp['hgrn_w_in'].reshape(2, 8, 128, 4, 8, 128)
    out['hgrn_w_in'] = c(a.transpose(0, 4, 2, 1, 3, 5)).reshape(2, 8, 128, 2, 2048)
    out['attn_w_o'] = inp['attn_w_o']
    out['hgrn_w_o'] = inp['hgrn_w_o']
    return out


def run_groups(inp, groups, cores=range(8), stop_after=None, xT0=None):
    cores = list(cores)
    inp = {k: np.ascontiguousarray(np.asarray(v, dtype=np.float32)) for k, v in inp.items()}
    sh = shared_consts(inp)
    base = host_weights(inp)
    base.update(sh)
    vec = [host_consts(inp, b) for b in cores]
    xT = [np.ascontiguousarray(inp['x'][b].T) for b in cores] if xT0 is None else xT0
    for layers in groups:
        nc = _get_nc(layers, stop_after)
        in_maps = []
        for i, b in enumerate(cores):
            m = dict(base)
            m['vecs'] = vec[i]
            m['xin'] = xT[i]
            in_maps.append(m)
        res = run_bass_kernel_spmd(nc, in_maps, core_ids=list(range(len(cores))))
        xT = [np.asarray(r['outT']) for r in res.results]
    return xT


def kernel(**inputs):
    xT = run_groups(inputs, LAUNCH_GROUPS)
    out = np.stack([np.ascontiguousarray(t.T) for t in xT], axis=0).astype(np.float32)
    return out
```

```python
import math
import numpy as np
import concourse.bass as bass
import concourse.mybir as mybir
from concourse.bass_utils import run_bass_kernel_spmd

F32 = mybir.dt.float32
BF16 = mybir.dt.bfloat16
AF = mybir.ActivationFunctionType
ALU = mybir.AluOpType

DEPTH = 4
D = 1024
S = 2048
DFF = 2816
EPS = 1e-6
NB = 4
TB = 512
ENG = ['pe', 'act', 'dve', 'pool', 'sp']
BLKNAME = {'pe': 'tensor', 'act': 'scalar', 'dve': 'vector', 'pool': 'gpsimd', 'sp': 'sync'}
RING = 4
SLOT = 6144
NSC = 18


class St:
    __slots__ = ('w', 'r', 'rd')

    def __init__(self):
        self.w = None
        self.r = {}
        self.rd = []


class T:
    def __init__(self, ap, st=None):
        self.ap = ap
        self.st = st or St()

    def v(self, ap):
        return T(ap, self.st)

    def __getitem__(self, key):
        return T(self.ap[key], self.st)


class Op:
    __slots__ = ('eng', 'fn', 'deps', 'sig', 'sem', 'val', 'isdma')

    def __init__(self, eng, fn, dsem):
        self.eng = eng
        self.fn = fn
        self.deps = []
        self.sig = False
        self.sem = dsem
        self.val = 0
        self.isdma = dsem is not None


class Rec:
    def __init__(self, nc):
        self.nc = nc
        self.ops = {e: [] for e in ENG}
        self.all = []
        self.esem = {e: nc.alloc_semaphore("es_" + e) for e in ENG}
        self.dry = False
        self.nsem = 0

    def newsem(self):
        self.nsem += 1
        return self.nc.alloc_semaphore("ds_%d" % self.nsem)

    def op(self, eng, fn, reads=(), writes=(), dsem=None):
        if self.dry:
            return None
        o = Op(eng, fn, dsem)
        deps = []
        for t in reads:
            if t.st.w is not None:
                deps.append(t.st.w)
        for t in writes:
            if t.st.w is not None:
                deps.append(t.st.w)
            deps.extend(t.st.r.values())
            deps.extend(t.st.rd)
        seen = set()
        for d in deps:
            if d is o or id(d) in seen:
                continue
            seen.add(id(d))
            if d.eng == 'pe' and eng == 'pe' and not d.isdma and not o.isdma:
                continue
            o.deps.append(d)
        for t in writes:
            t.st.w = o
            t.st.r = {}
            t.st.rd = []
        for t in reads:
            if t.st.w is o:
                continue
            if o.isdma:
                t.st.rd.append(o)
            else:
                t.st.r[eng] = o
        self.ops[eng].append(o)
        self.all.append(o)
        return o

    def mm(self, out, lhsT, rhs, start=True, stop=True, extra_reads=()):
        return self.op('pe', lambda e: e.matmul(out.ap, lhsT.ap, rhs.ap, start=start, stop=stop),
                       [lhsT, rhs] + list(extra_reads), [out])

    def tr(self, out, in_, ident):
        return self.op('pe', lambda e: e.transpose(out.ap, in_.ap, ident.ap), [in_, ident], [out])

    def act(self, out, in_, func, bias=0.0, scale=1.0):
        reads = [in_]
        b = bias
        s = scale
        if isinstance(bias, T):
            reads.append(bias)
            b = bias.ap
        if isinstance(scale, T):
            reads.append(scale)
            s = scale.ap
        return self.op('act', lambda e: e.activation(out.ap, in_.ap, func, bias=b, scale=s), reads, [out])

    def tt(self, eng, out, in0, in1, op):
        return self.op(eng, lambda e: e.tensor_tensor(out.ap, in0.ap, in1.ap, op), [in0, in1], [out])

    def ts(self, eng, out, in0, s1, s2, op0, op1=None):
        reads = [in0]
        a1 = s1
        a2 = s2
        if isinstance(s1, T):
            reads.append(s1)
            a1 = s1.ap
        if isinstance(s2, T):
            reads.append(s2)
            a2 = s2.ap
        if op1 is None:
            return self.op(eng, lambda e: e.tensor_scalar(out.ap, in0.ap, a1, None, op0), reads, [out])
        return self.op(eng, lambda e: e.tensor_scalar(out.ap, in0.ap, a1, a2, op0, op1), reads, [out])

    def stt(self, out, in0, scalar, in1, op0, op1):
        reads = [in0, in1]
        sc = scalar
        if isinstance(scalar, T):
            reads.append(scalar)
            sc = scalar.ap
        return self.op('dve', lambda e: e.scalar_tensor_tensor(out.ap, in0.ap, sc, in1.ap, op0, op1), reads, [out])

    def copy(self, eng, out, in_):
        if eng == 'act':
            return self.op('act', lambda e: e.copy(out.ap, in_.ap), [in_], [out])
        return self.op(eng, lambda e: e.tensor_copy(out.ap, in_.ap), [in_], [out])

    def memset(self, eng, out, val):
        return self.op(eng, lambda e: e.memset(out.ap, val), [], [out])

    def dma(self, q, out, in_, sem, reads=(), writes=()):
        oa = out.ap if isinstance(out, T) else out
        ia = in_.ap if isinstance(in_, T) else in_
        rd = list(reads) + ([in_] if isinstance(in_, T) else [])
        wr = list(writes) + ([out] if isinstance(out, T) else [])
        return self.op(q, lambda e: e.dma_start(out=oa, in_=ia), rd, wr, dsem=sem)

    def waitfor(self, eng, ops):
        if self.dry:
            return
        o = Op(eng, None, None)
        o.deps = [d for d in ops if d is not None]
        self.ops[eng].append(o)
        self.all.append(o)

    def emit(self):
        nc = self.nc
        for o in self.all:
            for d in o.deps:
                d.sig = True
        cnt = {}
        for o in self.all:
            if o.isdma:
                k = id(o.sem)
                cnt[k] = cnt.get(k, 0) + 16
                o.val = cnt[k]
        for e in ENG:
            c = 0
            for o in self.ops[e]:
                if o.isdma or o.fn is None:
                    continue
                if o.sig:
                    c += 1
                    o.val = c
                    o.sem = self.esem[e]
        with nc.Block() as blk:
            for e in ENG:
                def body(engine, e=e):
                    waited = {}
                    for o in self.ops[e]:
                        for d in o.deps:
                            k = id(d.sem)
                            if waited.get(k, 0) < d.val:
                                engine.wait_ge(d.sem, d.val)
                                waited[k] = d.val
                        if o.fn is None:
                            continue
                        ins = o.fn(engine)
                        if o.isdma:
                            ins.then_inc(o.sem, 16)
                        elif o.sig:
                            ins.then_inc(o.sem, 1)
                getattr(blk, BLKNAME[e])(body)


def vec_layout():
    lay = {}
    pos = [0]

    def add(name, w):
        lay[name] = (pos[0], w)
        pos[0] += w
    add('c', 8)
    for l in range(DEPTH):
        add('adab%d' % l, 72)
        add('ng%d' % l, 24)
    for j in range(2):
        add('gq%d' % j, 2)
        add('gk%d' % j, 1)
        add('subg%d' % j, 1)
        add('lam%d' % j, 4)
        add('og%d' % j, 8)
        add('lbl%d' % j, 8)
    add('b31', 8)
    return lay, pos[0]


def t5_bucket(d):
    d = np.maximum(d, 0)
    df = np.maximum(d, 1).astype(np.float32)
    large = 16 + (np.log(df / 16) / math.log(8) * 16).astype(np.int32)
    large = np.minimum(large, 31)
    return np.where(d < 16, d, large)


def fm(v):
    return np.ascontiguousarray(v.reshape(-1, 128).T)


def build(layers, stop_after=None):
    nc = bass.Bass("TRN2", target_bir_lowering=False)
    R = Rec(nc)
    lay, NV = vec_layout()

    def din(name, shape, dt=F32):
        return nc.dram_tensor(name, list(shape), dt, kind="ExternalInput").ap()

    xin = din("xin", [D, S])
    outT = nc.dram_tensor("outT", [D, S], F32, kind="ExternalOutput").ap()
    ada_w = din("ada_w", [DEPTH, 12, 128, 3, 2048])
    w_in = din("ffn_w_in", [DEPTH, 2, 11, 128, 2, 2048])
    w_down = din("ffn_w_down", [DEPTH, 2, 11, 128, 2, 1024])
    w_qkv = din("attn_w_qkv", [2, 8, 128, 2, 1536])
    w_ao = din("attn_w_o", [2, D, D])
    w_hin = din("hgrn_w_in", [2, 8, 128, 2, 2048])
    w_ho = din("hgrn_w_o", [2, D, D])
    vecs_d = din("vecs", [128, NV])
    biasg_d = din("biasg", [8, 128, 256])
    cmask_d = din("cmask", [128, 256])
    reset_d = din("resetm", [128, 512], BF16)
    reset16_d = din("reset16", [128, 512], BF16)
    caus_d = din("caus64", [64, 64])
    ident_d = din("ident", [128, 128])

    def sb(name, shape, dt):
        return nc.alloc_sbuf_tensor(name, list(shape), dt)

    xT_t = sb("xT", [128, 8, S], F32)
    hT_t = sb("hT", [128, 8, S], BF16)
    X = [[T(xT_t[:, c, n * TB:(n + 1) * TB]) for n in range(NB)] for c in range(8)]
    HT = [[T(hT_t[:, c, n * TB:(n + 1) * TB]) for n in range(NB)] for c in range(8)]
    ring_t = [sb("ring%d" % i, [128, SLOT], BF16) for i in range(RING)]
    ringP = [[T(ring_t[i][:, p * 1024:(p + 1) * 1024]) for p in range(6)] for i in range(RING)]
    semP = [[R.newsem() for p in range(6)] for _ in range(RING)]

    def RW(s, view):
        return ringP[s][0].v(view), ringP[s][1:6]
    SC = [T(sb("sc%d" % i, [128, 512], F32)[:, :]) for i in range(NSC)]
    BG_t = [sb("bg%d" % i, [128, S], BF16) for i in range(4)]
    QN = [T(BG_t[0][:, n * TB:(n + 1) * TB]) for n in range(NB)]
    QN1 = [T(BG_t[3][:, n * TB:(n + 1) * TB]) for n in range(NB)]
    KN = [T(BG_t[1][:, n * TB:(n + 1) * TB]) for n in range(NB)]
    VT = [T(BG_t[2][:, n * TB:(n + 1) * TB]) for n in range(NB)]
    vecs = T(sb("vecs_s", [128, NV], F32)[:, :])
    resetm = T(sb("resetm_s", [128, 512], BF16)[:, :])
    reset16 = T(sb("reset16_s", [128, 512], BF16)[:, :])
    cmask = T(sb("cmask_s", [128, 256], F32)[:, :])
    caus = T(sb("caus_s", [64, 64], F32)[:, :])
    ident = T(sb("ident_s", [128, 128], F32)[:, :])
    ones_bf = T(sb("ones_bf", [128, 128], BF16)[:, :])
    ones_f = T(sb("ones_f", [128, 128], F32)[:, :])
    bd_bf = T(sb("bd_bf", [128, 128], BF16)[:, :])
    biasT = [T(sb("biasT%d" % i, [128, 256], F32)[:, :]) for i in range(1)]
    BTt = [T(sb("BT%d" % i, [128, 256], F32)[:, :]) for i in range(1)]
    modT2 = [T(sb("modT%d" % i, [128, 72], F32)[:, :]) for i in range(2)]
    gs2 = [T(sb("gs%d" % i, [128, 24], F32)[:, :]) for i in range(2)]
    gt2 = [T(sb("gt%d" % i, [128, 24], F32)[:, :]) for i in range(2)]
    cact = T(sb("cact", [128, 8], BF16)[:, :])
    lbv = T(sb("lbv", [128, 8], F32)[:, :])
    oml = T(sb("oml", [128, 8], F32)[:, :])
    neglam = T(sb("neglam", [128, 4], F32)[:, :])
    lamt = T(sb("lamt", [128, 4], F32)[:, :])
    Sst = T(sb("Sst", [128, 128], F32)[:, :])
    Sbf2 = [T(sb("Sbf%d" % i, [128, 128], BF16)[:, :]) for i in range(2)]
    PS = [T(nc.alloc_psum_tensor("ps%d" % i, [128, 512], F32)[:, :]) for i in range(8)]

    def V(name, lo=0, w=None):
        s0, ww = lay[name]
        if w is None:
            w = ww - lo
        return vecs[:, s0 + lo:s0 + lo + w]

    class WRing:
        def __init__(self):
            self.jobs = []
            self.pos = 0
            self.issued = 0

        def _issue(self, k):
            s = k % RING
            for p0, p1, src, dstfn in self.jobs[k]:
                R.dma('pool', ringP[s][p0].v(dstfn(ring_t[s])), src, semP[s][p0], writes=ringP[s][p0 + 1:p1])

        def next(self, spec):
            if R.dry:
                self.jobs.append(spec)
                return 0
            k = self.pos
            self.pos += 1
            while self.issued < min(k + RING - 1, len(self.jobs)):
                self._issue(self.issued)
                self.issued += 1
            return k % RING

    W = WRing()

    def setup():
        sx = [R.newsem() for _ in range(8)]
        for c in range(8):
            R.dma('sp', T(xT_t[:, c, :]), xin[c * 128:(c + 1) * 128, :], sx[c], writes=X[c])
        for dst, src in ((vecs, vecs_d), (resetm, reset_d), (reset16, reset16_d), (cmask, cmask_d), (caus, caus_d), (ident, ident_d)):
            R.dma('sp', dst, src, R.newsem())
        R.memset('dve', ones_bf, 1.0)
        R.memset('dve', ones_f, 1.0)
        R.memset('dve', bd_bf, 0.0)
        R.memset('dve', bd_bf[0:64, 0:64], 1.0)
        R.memset('dve', bd_bf[64:128, 64:128], 1.0)
        R.act(cact, V('c'), AF.Silu)

    def ada_tasks(l):
        modT, gs, gt = modT2[l % 2], gs2[l % 2], gt2[l % 2]
        tasks = []
        for q in range(12):
            def task(q=q):
                src = ada_w[l, q]
                s = W.next([(0, 6, src, lambda rt: rt[:, :].rearrange("p (a n) -> p a n", a=3))])
                if R.dry:
                    return
                wv = ring_t[s][:, :].rearrange("p (kc n) -> p kc n", kc=8)
                for cc in range(6):
                    col = q * 6 + cc
                    for kc in range(8):
                        lt, er = RW(s, wv[:, kc, cc * 128:(cc + 1) * 128])
                        R.mm(PS[7][:, col:col + 1], lt, cact[:, kc:kc + 1], start=(kc == 0), stop=(kc == 7),
                             extra_reads=er)
            tasks.append(task)

        def fin():
            R.tt('dve', modT, PS[7][:, 0:72], V('adab%d' % l), ALU.add)
            for s in range(3):
                R.stt(gs[:, s * 8:(s + 1) * 8], modT[:, s * 24 + 8:s * 24 + 16], 1.0, V('ng%d' % l, s * 8, 8),
                      ALU.add, ALU.mult)
                R.ts('dve', gt[:, s * 8:(s + 1) * 8], modT[:, s * 24 + 16:s * 24 + 24], 0.5 if s != 1 else 1.0, None,
                     ALU.mult)
        tasks.append(fin)
        return tasks

    def norm(l, s):
        modT, gs = modT2[l % 2], gs2[l % 2]
        sq_eng = ['dve', 'pool', 'dve', 'pool', 'dve', 'pool', 'dve', 'dve']
        for n in range(NB):
            for c in range(8):
                sq = SC[c % 2].v(SC[c % 2].ap.bitcast(BF16)[:, (n % 2) * 512:(n % 2 + 1) * 512])
                R.tt(sq_eng[c], sq, X[c][n], X[c][n], ALU.mult)
                R.mm(PS[6], ones_bf, sq, start=(c == 0), stop=(c == 7))
            lnv = SC[2]
            rstd = SC[3 + n % 2]
            R.act(lnv, PS[6], AF.Ln, bias=EPS, scale=1.0 / D)
            R.act(rstd, lnv, AF.Exp, scale=-0.5)
            for c in range(8):
                tmp = SC[5 + c % 2]
                R.tt('dve', tmp, X[c][n], rstd, ALU.mult)
                R.act(HT[c][n], tmp, AF.Identity, bias=modT[:, s * 24 + c:s * 24 + c + 1],
                      scale=gs[:, s * 8 + c:s * 8 + c + 1])

    def ffn(l, s, inject=None):
        gt = gt2[l % 2]
        inject = inject or []
        fi = 0 if s == 0 else 1
        pending = []
        step = 0
        for g in range(11):
            sl = W.next([(0, 4, w_in[l, fi, g], lambda rt: rt[:, 0:4096].rearrange("p (a n) -> p a n", a=2)),
                         (4, 6, w_down[l, fi, g], lambda rt: rt[:, 4096:SLOT].rearrange("p (m d) -> p m d", m=2))])
            if R.dry:
                if inject:
                    inject.pop(0)()
                continue
            wab = ring_t[sl][:, 0:4096].rearrange("p (kc ab f) -> p kc ab f", kc=8, ab=2)
            wa0 = wab[:, :, 0, :]
            wa1 = wab[:, :, 1, :]
            wb = ring_t[sl][:, 4096:SLOT].rearrange("p (m d) -> p m d", m=2)
            for n in range(NB):
                us = []
                for m in range(2):
                    pa = PS[(step % 2) * 2]
                    pb = PS[(step % 2) * 2 + 1]
                    for kc in range(8):
                        lt, er = RW(sl, wa0[:, kc, m * 128:(m + 1) * 128])
                        R.mm(pa, lt, HT[kc][n], start=(kc == 0), stop=(kc == 7), extra_reads=er)
                    for kc in range(8):
                        lt, er = RW(sl, wa1[:, kc, m * 128:(m + 1) * 128])
                        R.mm(pb, lt, HT[kc][n], start=(kc == 0), stop=(kc == 7), extra_reads=er)
                    silt = SC[7 + step % 2]
                    bt = SC[9 + step % 2]
                    R.act(silt, pa, AF.Silu)
                    R.copy('act', bt, pb)
                    ut = SC[11 + (step % 4)]
                    u = ut.v(ut.ap.bitcast(BF16)[:, 0:512])
                    R.tt('pool', u, silt, bt, ALU.mult)
                    us.append(u)
                    step += 1
                    if len(pending) > (1 if m == 0 else 0) and pending:
                        pending.pop(0)()

                def mk_down(j0, us=us, n=n, sl=sl, wb=wb):
                    def down():
                        for j in range(j0, j0 + 4):
                            py = PS[4 + j % 3]
                            for m in range(2):
                                lt, er = RW(sl, wb[:, m, j * 128:(j + 1) * 128])
                                R.mm(py, lt, us[m], start=(m == 0), stop=(m == 1), extra_reads=er)
                            R.stt(X[j][n], py, gt[:, s * 8 + j:s * 8 + j + 1], X[j][n], ALU.mult, ALU.add)
                    return down
                pending.append(mk_down(0))
                pending.append(mk_down(4))
            if inject:
                inject.pop(0)()
                while pending:
                    pending.pop(0)()
        while pending:
            pending.pop(0)()
        while inject:
            inject.pop(0)()

    def attn(l):
        gt = gt2[l % 2]
        j = l // 2
        lam_init = 0.8 - 0.6 * math.exp(-0.3 * l)
        R.tt('dve', lamt[:, 0:1], V('lam%d' % j, 0, 1), V('lam%d' % j, 1, 1), ALU.mult)
        R.tt('dve', lamt[:, 1:2], V('lam%d' % j, 2, 1), V('lam%d' % j, 3, 1), ALU.mult)
        R.mm(PS[7][:, 0:2], ones_f, lamt[:, 0:2])
        R.act(lamt[:, 2:4], PS[7][:, 0:2], AF.Exp)
        R.tt('dve', neglam[:, 0:1], lamt[:, 3:4], lamt[:, 2:3], ALU.subtract)
        R.ts('dve', neglam[:, 1:2], neglam[:, 0:1], -lam_init, None, ALU.add)
        NL = neglam[:, 1:2]
        st = {'tix': 0, 'pidx': 0}
        epi = []
        for h in range(8):
            srcB = w_ao[j][h * 128:(h + 1) * 128, :]
            spec = [(0, 3, w_qkv[j, h], lambda rt: rt[:, 0:3072].rearrange("p (a n) -> p a n", a=2)),
                    (3, 4, srcB, lambda rt: rt[:, 3072:4096])]
            sl = W.next(spec)
            if R.dry:
                continue
            wa = ring_t[sl][:, 0:3072].rearrange("p (kc t f) -> p kc t f", kc=8, t=3)
            wo = ring_t[sl][:, 3072:4096]
            bT = biasT[0]
            BT = BTt[0]
            R.dma('sp', bT, biasg_d[h], R.newsem())
            R.tt('pool', BT, bT, cmask, ALU.add)
            b31 = V('b31', h, 1)
            tails = []
            kidx = 0
            for which, dstl, gain, lnb in ((0, QN, V('gq%d' % j, 0, 1), math.log(0.125)), (1, KN, V('gk%d' % j), 0.0)):
                for n in range(NB):
                    pq = PS[kidx % 2]
                    for kc in range(8):
                        lt, er = RW(sl, wa[:, kc, which, :])
                        R.mm(pq, lt, HT[kc][n], start=(kc == 0), stop=(kc == 7), extra_reads=er)
                    qs = SC[kidx % 2]
                    R.copy('act', qs, pq)
                    sq = SC[2].v(SC[2].ap.bitcast(BF16)[:, (kidx % 2) * 512:(kidx % 2 + 1) * 512])
                    R.tt('dve', sq, qs, qs, ALU.mult)

                    def tail(which=which, dstl=dstl, gain=gain, lnb=lnb, n=n, qs=qs, sq=sq, k2=kidx % 2):
                        R.mm(PS[2 + k2], bd_bf, sq)
                        lnv = SC[3].v(SC[3].ap[:, :]) if k2 == 0 else SC[9].v(SC[9].ap[:, :])
                        rs = SC[4].v(SC[4].ap[:, :]) if k2 == 0 else SC[10].v(SC[10].ap[:, :])
                        R.act(lnv, PS[2 + k2], AF.Ln, bias=EPS, scale=1.0 / 64)
                        R.act(rs, lnv, AF.Exp, bias=lnb, scale=-0.5)
                        R.stt(dstl[n], qs, gain, rs, ALU.mult, ALU.mult)
                        if which == 0:
                            R.stt(QN1[n], qs, V('gq%d' % j, 1, 1), rs, ALU.mult, ALU.mult)
                    tails.append(tail)
                    if len(tails) > 1:
                        tails.pop(0)()
                    kidx += 1
                    if epi:
                        epi.pop(0)()
            for tt_ in range(16):
                pv = PS[4 + (tt_ // 4) % 2]
                for kc in range(8):
                    lt, er = RW(sl, wa[:, kc, 2, :])
                    R.mm(pv[:, (tt_ % 4) * 128:(tt_ % 4 + 1) * 128],
                         HT[kc][tt_ // 4].v(hT_t[:, kc, tt_ * 128:(tt_ + 1) * 128]),
                         lt, start=(kc == 0), stop=(kc == 7), extra_reads=er)
                if tt_ % 4 == 3:
                    R.copy('dve', VT[tt_ // 4], pv)
                    while tails:
                        tails.pop(0)()
            for jq in range(NB):
                nk = 4 * jq + 4
                pend = []
                nd = list(range(0, max(4 * jq - 1, 0)))
                dg = list(range(max(4 * jq - 1, 0), nk))
                order = []
                while nd or dg:
                    if nd:
                        order.append(nd.pop(0))
                    if dg and (order or not nd):
                        order.append(dg.pop(0))
                for oi, i in enumerate(order):
                    delta = 128 * i - 512 * jq
                    c0 = max(delta, 0)
                    diag = i >= 4 * jq - 1
                    for c in range(2):
                        ps = PS[st['tix'] % 3]
                        st['tix'] += 1
                        kt = KN[i // 4].v(BG_t[1][:, i * 128:(i + 1) * 128])
                        qsrc = (QN, QN1)[c][jq]
                        qt = qsrc.v(BG_t[0 if c == 0 else 3][:, jq * 512 + c0:(jq + 1) * 512])
                        R.mm(ps[:, c0:512], kt, qt)
                        pk = st['pidx'] % 6
                        ptile = SC[5 + pk % 3]
                        st['pidx'] += 1
                        pt = ptile.v(ptile.ap.bitcast(BF16)[:, (pk // 3) * 512:(pk // 3 + 1) * 512])
                        if not diag:
                            R.act(pt, ps, AF.Exp, bias=b31)
                        else:
                            cb1 = min(delta + 256, 512)
                            tmp = SC[9 + c]
                            R.tt('dve', tmp[:, c0:cb1], ps[:, c0:cb1], BT[:, c0 - delta:cb1 - delta], ALU.add)
                            R.act(pt[:, c0:cb1], tmp[:, c0:cb1], AF.Exp)
                            if cb1 < 512:
                                R.act(pt[:, cb1:512], ps[:, cb1:512], AF.Exp, bias=b31)

                        def av(i=i, c=c, pt=pt, c0=c0, first=(oi == 0), last=(oi == nk - 1)):
                            vt = VT[i // 4][:, (i % 4) * 128:(i % 4 + 1) * 128]
                            R.mm(PS[4 + c][:, c0:512], vt, pt[:, c0:512], start=first, stop=last)
                            R.mm(PS[6 + c][:, c0:512], ones_bf, pt[:, c0:512], start=first, stop=last)
                        pend.append(av)
                        if len(pend) > 2:
                            pend.pop(0)()
                        if epi:
                            epi.pop(0)()
                while pend:
                    pend.pop(0)()
                r0, r1, t0, t1, ob = SC[11], SC[12], SC[13], SC[14], SC[15]
                R.op('dve', lambda e, a=r0, b=PS[6]: e.reciprocal(a.ap, b.ap), [PS[6]], [r0])
                R.copy('act', t0, PS[4])
                R.op('dve', lambda e, a=r1, b=PS[7]: e.reciprocal(a.ap, b.ap), [PS[7]], [r1])
                R.copy('act', t1, PS[5])
                R.tt('dve', t0, t0, r0, ALU.mult)
                R.tt('pool', t1, t1, r1, ALU.mult)
                R.stt(ob, t1, NL, t0, ALU.mult, ALU.add)
                sq = SC[8].v(SC[8].ap.bitcast(BF16)[:, 0:512])
                R.tt('pool', sq, ob, ob, ALU.mult)
                oh = SC[8].v(SC[8].ap.bitcast(BF16)[:, 512:1024])

                def stage_b(sq=sq, ob=ob, oh=oh):
                    R.mm(PS[3], ones_bf, sq)
                    R.act(SC[11], PS[3], AF.Ln, bias=EPS, scale=1.0 / 128)
                    R.act(SC[12], SC[11], AF.Exp, bias=math.log(1.0 - lam_init), scale=-0.5)
                    R.stt(oh, ob, V('subg%d' % j), SC[12], ALU.mult, ALU.mult)
                epi.append(stage_b)
                for jj in range(8):
                    def stage_c(jj=jj, jq=jq, oh=oh, sl=sl, wo=wo):
                        lt, er = RW(sl, wo[:, jj * 128:(jj + 1) * 128])
                        R.mm(PS[3], lt, oh, extra_reads=er)
                        R.stt(X[jj][jq], PS[3], gt[:, 8 + jj:8 + jj + 1], X[jj][jq], ALU.mult, ALU.add)
                    epi.append(stage_c)
        while epi:
            epi.pop(0)()

    def hgrn(l):
        gt = gt2[l % 2]
        j = l // 2
        if j == 0:
            R.memset('dve', lbv, 0.0)
        else:
            R.tt('dve', lbv, V('lbl1'), V('lbl0'), ALU.subtract)
            R.act(lbv, lbv, AF.Sigmoid)
        R.ts('dve', oml, lbv, -1.0, 1.0, ALU.mult, ALU.add)
        sig, fg, lg, G, G16, eG, kk, kh, gsig, gsilu = SC[0:10]
        eL = gsig
        qq = SC[10].v(SC[10].ap.bitcast(BF16))
        Qs, qh = qq[:, 0:512], qq[:, 512:1024]
        KA = SC[11].v(SC[11].ap.bitcast(BF16))
        KB = SC[14].v(SC[14].ap.bitcast(BF16))
        Kp = [KA[:, 0:512], KA[:, 512:1024], KB[:, 0:512], KB[:, 512:1024]]
        vT = SC[12].v(SC[12].ap.bitcast(BF16).rearrange("p (c d) -> p c d", c=8)[0:64])
        khT = SC[13].v(SC[13].ap.bitcast(BF16).rearrange("p (c d) -> p c d", c=8)[0:64])
        BGH = [T(BG_t[k // 2][:, :].bitcast(F32)[:, (k % 2) * 512:(k % 2 + 1) * 512]) for k in range(8)]
        E = [SC[16], SC[17], BGH[5], BGH[6]]
        gsilu2 = [gsilu, BGH[0]]
        osb, lnv, tt_o = BGH[1], BGH[3], BGH[4]
        sqo = BGH[2].v(BGH[2].ap.bitcast(BF16))
        sq, oh = sqo[:, 0:512], sqo[:, 512:1024]
        outq = []
        blk = [0]

        def popq(k=1):
            for _ in range(k):
                if outq:
                    outq.pop(0)()
        for e_ in E:
            R.memset('pool', e_, 0.0)
        R.memset('dve', PS[7], 0.0)
        for h in range(8):
            srcB = w_ho[j][h * 128:(h + 1) * 128, :]
            spec = [(0, 4, w_hin[j, h], lambda rt: rt[:, 0:4096].rearrange("p (a n) -> p a n", a=2)),
                    (4, 5, srcB, lambda rt: rt[:, 4096:5120])]
            sl = W.next(spec)
            if R.dry:
                continue
            wa = ring_t[sl][:, 0:4096].rearrange("p (kc t f) -> p kc t f", kc=8, t=4)
            wo = ring_t[sl][:, 4096:5120]
            R.memset('dve', Sst, 0.0)
            R.memset('pool', Sbf2[0], 0.0)
            og = V('og%d' % j, h, 1)
            for n in range(NB):
                gsl = gsilu2[blk[0] % 2]
                blk[0] += 1
                for kc in range(8):
                    lt, er = RW(sl, wa[:, kc, 1, :])
                    R.mm(PS[0], lt, HT[kc][n], start=(kc == 0), stop=(kc == 7), extra_reads=er)
                R.act(sig, PS[0], AF.Sigmoid)
                for kc in range(8):
                    lt, er = RW(sl, wa[:, kc, 3, :])
                    R.mm(PS[2], lt, HT[kc][n], start=(kc == 0), stop=(kc == 7), extra_reads=er)
                for kc in range(8):
                    lt, er = RW(sl, wa[:, kc, 0, :])
                    R.mm(PS[1], lt, HT[kc][n], start=(kc == 0), stop=(kc == 7), extra_reads=er)
                for ci in range(8):
                    pv = PS[3 + ci // 4]
                    for kc in range(8):
                        lt, er = RW(sl, wa[:, kc, 2, :])
                        R.mm(pv[0:64, (ci % 4) * 128:(ci % 4 + 1) * 128],
                             HT[kc][n].v(hT_t[:, kc, n * 512 + ci * 64:n * 512 + ci * 64 + 64]),
                             lt, start=(kc == 0), stop=(kc == 7), extra_reads=er)
                R.act(gsig, PS[2], AF.Sigmoid)
                R.tt('dve', gsl, PS[2], gsig, ALU.mult)
                popq()
                R.ts('dve', fg, sig, oml[:, h:h + 1], lbv[:, h:h + 1], ALU.mult, ALU.add)
                R.act(lg, fg, AF.Ln)
                R.ts('pool', kk, fg, -1.0, 1.0, ALU.mult, ALU.add)
                popq()
                R.op('dve', lambda e, o_=G, d1=lg: e.tensor_tensor_scan(o_.ap, resetm.ap, d1.ap, 0.0, ALU.mult, ALU.add),
                     [resetm, lg], [G])
                R.op('dve', lambda e, o_=G16, d1=lg: e.tensor_tensor_scan(o_.ap, reset16.ap, d1.ap, 0.0, ALU.mult, ALU.add),
                     [reset16, lg], [G16])
                R.act(eG, G, AF.Exp)
                R.act(G16, G16, AF.Exp)
                popq()
                Gv = G.ap.rearrange("p (c t) -> p c t", c=8)
                for i_ in range(4):
                    w_ = 16 * (i_ + 1)
                    Ev = E[i_].v(E[i_].ap.rearrange("p (c t) -> p c t", c=8)[:, :, 0:w_])
                    if i_ == 0:
                        R.act(Ev, G.v(Gv[:, :, 0:w_]), AF.Exp, scale=-1.0)
                    else:
                        R.tt('dve', Ev, G.v(Gv[:, :, 16 * i_ - 1:16 * i_].to_broadcast([128, 8, w_])),
                             G.v(Gv[:, :, 0:w_]), ALU.subtract)
                        R.act(Ev, Ev, AF.Exp)
                R.tt('dve', eL, G.v(Gv[:, :, 63:64].to_broadcast([128, 8, 64])), G.v(Gv), ALU.subtract)
                R.act(eL, eL, AF.Exp)
                popq()
                R.tt('dve', Qs, PS[1], G16, ALU.mult)
                R.tt('dve', qh, PS[1], eG, ALU.mult)
                for i_ in range(4):
                    R.tt('dve' if i_ < 2 else 'pool', Kp[i_], kk, E[i_], ALU.mult)
                R.tt('pool', kh, kk, eL, ALU.mult)
                popq()
                R.copy('act', vT[:, 0:4, :], PS[3].v(PS[3].ap[0:64, :].rearrange("p (c d) -> p c d", c=4)))
                R.copy('act', vT[:, 4:8, :], PS[4].v(PS[4].ap[0:64, :].rearrange("p (c d) -> p c d", c=4)))
                popq()
                for ci in range(8):
                    pt_ = PS[5 + ci // 4]
                    R.tr(pt_[0:64, (ci % 4) * 128:(ci % 4 + 1) * 128], kh[:, ci * 64:ci * 64 + 64], ident)
                R.copy('dve', khT[:, 0:4, :], PS[5].v(PS[5].ap[0:64, :].rearrange("p (c d) -> p c d", c=4)))
                R.copy('dve', khT[:, 4:8, :], PS[6].v(PS[6].ap[0:64, :].rearrange("p (c d) -> p c d", c=4)))
                popq()
                PSO = PS[2]
                AbfAll = SC[15].v(SC[15].ap.bitcast(BF16)[0:64, 512:1024])
                for ci in range(8):
                    cs = ci * 64
                    for i_ in range(4):
                        w_ = 16 * (i_ + 1)
                        R.mm(PS[7][0:w_, cs + 16 * i_:cs + 16 * i_ + 16], Kp[i_][:, cs:cs + w_],
                             Qs[:, cs + 16 * i_:cs + 16 * i_ + 16])
                R.tt('dve', AbfAll.v(AbfAll.ap.rearrange("p (c t) -> p c t", c=8)),
                     PS[7].v(PS[7].ap[0:64, :].rearrange("p (c t) -> p c t", c=8)),
                     caus.v(caus.ap.rearrange("p (o t) -> p o t", o=1).to_broadcast([64, 8, 64])), ALU.mult)
                popq()
                for ci in range(8):
                    pu = PS[ci // 4][:, (ci % 4) * 128:(ci % 4 + 1) * 128]
                    R.mm(pu, khT[:, ci, :], vT[:, ci, :])
                popq(2)
                for ci in range(8):
                    cs = ci * 64
                    pu = PS[ci // 4][:, (ci % 4) * 128:(ci % 4 + 1) * 128]
                    R.mm(PSO[:, cs:cs + 64], vT[:, ci, :], AbfAll[:, cs:cs + 64], start=True, stop=False)
                    R.mm(PSO[:, cs:cs + 64], Sbf2[ci % 2], qh[:, cs:cs + 64], start=False, stop=True)
                    R.stt(Sbf2[(ci + 1) % 2], Sst, eG[:, cs + 63:cs + 64], pu, ALU.mult, ALU.add)
                    R.stt(Sst, Sst, eG[:, cs + 63:cs + 64], pu, ALU.mult, ALU.add)
                R.copy('act', osb, PSO)

                def s0(gsl=gsl, og=og):
                    R.tt('pool', sq, osb, osb, ALU.mult)
                    R.mm(PS[6], ones_bf, sq)
                    R.act(lnv, PS[6], AF.Ln, bias=EPS, scale=1.0 / 128)
                    R.act(lnv, lnv, AF.Exp, scale=-0.5)
                    R.stt(tt_o, osb, og, lnv, ALU.mult, ALU.mult)
                    R.tt('pool', oh, tt_o, gsl, ALU.mult)
                outq.append(s0)
                for jj in range(8):
                    def sy(jj=jj, n=n, sl=sl, wo=wo):
                        lt, er = RW(sl, wo[:, jj * 128:(jj + 1) * 128])
                        R.mm(PS[6], lt, oh, extra_reads=er)
                        R.stt(X[jj][n], PS[6], gt[:, 8 + jj:8 + jj + 1], X[jj][n], ALU.mult, ALU.add)
                    outq.append(sy)
        popq(100)

    def finish():
        so = [R.newsem() for _ in range(8)]
        outs = []
        for c in range(8):
            outs.append(R.dma('sp', outT[c * 128:(c + 1) * 128, :], T(xT_t[:, c, :]), so[c], reads=X[c]))
        R.waitfor('sp', [o for o in R.all if o.isdma])

    def program():
        if not R.dry:
            setup()
        done = False
        for idx, l in enumerate(layers):
            if idx == 0:
                for t_ in ada_tasks(l):
                    t_()
            for s in range(3):
                if not R.dry:
                    norm(l, s)
                if s == 1:
                    if l % 2 == 0:
                        attn(l)
                    else:
                        hgrn(l)
                else:
                    inj = None
                    last = stop_after is not None and (l, s) == tuple(stop_after)
                    if s == 2 and idx + 1 < len(layers) and not last:
                        inj = ada_tasks(layers[idx + 1])
                    ffn(l, s, inj)
                if stop_after is not None and (l, s) == tuple(stop_after):
                    done = True
                    break
            if done:
                break
        if not R.dry:
            finish()

    R.dry = True
    program()
    R.dry = False
    program()
    R.emit()
    return nc


LAUNCH_GROUPS = [[0, 1, 2, 3]]
_NC_CACHE = {}


def _get_nc(layers, stop_after=None):
    key = (tuple(layers), stop_after)
    if key not in _NC_CACHE:
        _NC_CACHE[key] = build(list(layers), stop_after)
    return _NC_CACHE[key]


def host_consts(inp, b):
    lay, NV = vec_layout()
    vecs = np.zeros((128, NV), np.float32)

    def put(name, arr):
        s0, w = lay[name]
        vecs[:, s0:s0 + w] = arr.reshape(128, w)
    put('c', fm(inp['c'][b]))
    for l in range(DEPTH):
        put('adab%d' % l, fm(inp['ada_b'][l]))
        put('ng%d' % l, fm(inp['norm_g'][l].reshape(-1)))
    for j in range(2):
        gq = np.zeros((128, 2), np.float32)
        gq[0:64, 0] = inp['attn_q_gain'][j]
        gq[64:128, 1] = inp['attn_q_gain'][j]
        put('gq%d' % j, gq)
        put('gk%d' % j, np.tile(inp['attn_k_gain'][j], 2).reshape(128, 1))
        put('subg%d' % j, inp['attn_subln_gain'][j].reshape(128, 1))
        lam = np.zeros((128, 4), np.float32)
        lam[0:64, :] = inp['attn_lambda'][j].T
        put('lam%d' % j, lam)
        put('og%d' % j, fm(inp['hgrn_out_gain'][j]))
        put('lbl%d' % j, fm(inp['hgrn_lb_logits'][j]))
    put('b31', np.broadcast_to(inp['rel_bias'][31][None, :], (128, 8)).copy())
    return vecs


def shared_consts(inp):
    kk = np.arange(128)[:, None]
    e = np.arange(256)[None, :]
    dist = e - kk
    bidx = t5_bucket(dist)
    biasg = np.ascontiguousarray(np.transpose(inp['rel_bias'][bidx], (2, 0, 1))).astype(np.float32)
    cmask = np.where(dist >= 0, 0.0, -30000.0).astype(np.float32)
    resetm = np.ones((128, 512), np.float32)
    resetm[:, 0::64] = 0.0
    reset16 = np.ones((128, 512), np.float32)
    reset16[:, 0::16] = 0.0
    s_ = np.arange(64)[:, None]
    t_ = np.arange(64)[None, :]
    caus = (s_ <= t_).astype(np.float32)
    ident = np.eye(128, dtype=np.float32)
    import ml_dtypes
    return dict(biasg=biasg, cmask=cmask, resetm=resetm.astype(ml_dtypes.bfloat16), reset16=reset16.astype(ml_dtypes.bfloat16), caus64=caus, ident=ident)


WKEYS = ['ada_w', 'ffn_w_in', 'ffn_w_down', 'attn_w_qkv', 'attn_w_o', 'hgrn_w_in', 'hgrn_w_o']


def host_weights(inp):
    c = np.ascontiguousarray
    out = {}
    a = inp['ada_w'].reshape(DEPTH, 8, 128, 12, 768)
    out['ada_w'] = c(a.transpose(0, 3, 2, 1, 4)).reshape(DEPTH, 12, 128, 3, 2048)
    a = inp['ffn_w_in'].reshape(DEPTH, 2, 8, 128, 2, 11, 256)
    out['ffn_w_in'] = c(a.transpose(0, 1, 5, 3, 2, 4, 6)).reshape(DEPTH, 2, 11, 128, 2, 2048)
    a = inp['ffn_w_down'].reshape(DEPTH, 2, 11, 2, 128, 1024)
    out['ffn_w_down'] = c(a.transpose(0, 1, 2, 4, 3, 5))
    a = inp['attn_w_qkv'].reshape(2, 8, 128, 3, 8, 128)
    out['attn_w_qkv'] = c(a.transpose(0, 4, 2, 1, 3, 5)).reshape(2, 8, 128, 2, 1536)
    a = inp['hgrn_w_in'].reshape(2, 8, 128, 4, 8, 128)
    out['hgrn_w_in'] = c(a.transpose(0, 4, 2, 1, 3, 5)).reshape(2, 8, 128, 2, 2048)
    out['attn_w_o'] = inp['attn_w_o']
    out['hgrn_w_o'] = inp['hgrn_w_o']
    return out


def run_groups(inp, groups, cores=range(8), stop_after=None, xT0=None):
    cores = list(cores)
    inp = {k: np.ascontiguousarray(np.asarray(v, dtype=np.float32)) for k, v in inp.items()}
    sh = shared_consts(inp)
    base = host_weights(inp)
    base.update(sh)
    vec = [host_consts(inp, b) for b in cores]
    xT = [np.ascontiguousarray(inp['x'][b].T) for b in cores] if xT0 is None else xT0
    for layers in groups:
        nc = _get_nc(layers, stop_after)
        in_maps = []
        for i, b in enumerate(cores):
            m = dict(base)
            m['vecs'] = vec[i]
            m['xin'] = xT[i]
            in_maps.append(m)
        res = run_bass_kernel_spmd(nc, in_maps, core_ids=list(range(len(cores))))
        xT = [np.asarray(r['outT']) for r in res.results]
    return xT


def kernel(**inputs):
    xT = run_groups(inputs, LAUNCH_GROUPS)
    out = np.stack([np.ascontiguousarray(t.T) for t in xT], axis=0).astype(np.float32)
    return out
```

```python
import math
import numpy as np
import concourse.bass as bass
import concourse.mybir as mybir
from concourse.bass_utils import run_bass_kernel_spmd

F32 = mybir.dt.float32
BF16 = mybir.dt.bfloat16
AF = mybir.ActivationFunctionType
ALU = mybir.AluOpType

DEPTH = 4
D = 1024
S = 2048
DFF = 2816
EPS = 1e-6
NB = 4
TB = 512
ENG = ['pe', 'act', 'dve', 'pool', 'sp']
BLKNAME = {'pe': 'tensor', 'act': 'scalar', 'dve': 'vector', 'pool': 'gpsimd', 'sp': 'sync'}
RING = 4
SLOT = 6144
NSC = 18


class St:
    __slots__ = ('w', 'r', 'rd')

    def __init__(self):
        self.w = None
        self.r = {}
        self.rd = []


class T:
    def __init__(self, ap, st=None):
        self.ap = ap
        self.st = st or St()

    def v(self, ap):
        return T(ap, self.st)

    def __getitem__(self, key):
        return T(self.ap[key], self.st)


class Op:
    __slots__ = ('eng', 'fn', 'deps', 'sig', 'sem', 'val', 'isdma')

    def __init__(self, eng, fn, dsem):
        self.eng = eng
        self.fn = fn
        self.deps = []
        self.sig = False
        self.sem = dsem
        self.val = 0
        self.isdma = dsem is not None


class Rec:
    def __init__(self, nc):
        self.nc = nc
        self.ops = {e: [] for e in ENG}
        self.all = []
        self.esem = {e: nc.alloc_semaphore("es_" + e) for e in ENG}
        self.dry = False
        self.nsem = 0

    def newsem(self):
        self.nsem += 1
        return self.nc.alloc_semaphore("ds_%d" % self.nsem)

    def op(self, eng, fn, reads=(), writes=(), dsem=None):
        if self.dry:
            return None
        o = Op(eng, fn, dsem)
        deps = []
        for t in reads:
            if t.st.w is not None:
                deps.append(t.st.w)
        for t in writes:
            if t.st.w is not None:
                deps.append(t.st.w)
            deps.extend(t.st.r.values())
            deps.extend(t.st.rd)
        seen = set()
        for d in deps:
            if d is o or id(d) in seen:
                continue
            seen.add(id(d))
            if d.eng == 'pe' and eng == 'pe' and not d.isdma and not o.isdma:
                continue
            o.deps.append(d)
        for t in writes:
            t.st.w = o
            t.st.r = {}
            t.st.rd = []
        for t in reads:
            if t.st.w is o:
                continue
            if o.isdma:
                t.st.rd.append(o)
            else:
                t.st.r[eng] = o
        self.ops[eng].append(o)
        self.all.append(o)
        return o

    def mm(self, out, lhsT, rhs, start=True, stop=True, extra_reads=()):
        return self.op('pe', lambda e: e.matmul(out.ap, lhsT.ap, rhs.ap, start=start, stop=stop),
                       [lhsT, rhs] + list(extra_reads), [out])

    def tr(self, out, in_, ident):
        return self.op('pe', lambda e: e.transpose(out.ap, in_.ap, ident.ap), [in_, ident], [out])

    def act(self, out, in_, func, bias=0.0, scale=1.0):
        reads = [in_]
        b = bias
        s = scale
        if isinstance(bias, T):
            reads.append(bias)
            b = bias.ap
        if isinstance(scale, T):
            reads.append(scale)
            s = scale.ap
        return self.op('act', lambda e: e.activation(out.ap, in_.ap, func, bias=b, scale=s), reads, [out])

    def tt(self, eng, out, in0, in1, op):
        return self.op(eng, lambda e: e.tensor_tensor(out.ap, in0.ap, in1.ap, op), [in0, in1], [out])

    def ts(self, eng, out, in0, s1, s2, op0, op1=None):
        reads = [in0]
        a1 = s1
        a2 = s2
        if isinstance(s1, T):
            reads.append(s1)
            a1 = s1.ap
        if isinstance(s2, T):
            reads.append(s2)
            a2 = s2.ap
        if op1 is None:
            return self.op(eng, lambda e: e.tensor_scalar(out.ap, in0.ap, a1, None, op0), reads, [out])
        return self.op(eng, lambda e: e.tensor_scalar(out.ap, in0.ap, a1, a2, op0, op1), reads, [out])

    def stt(self, out, in0, scalar, in1, op0, op1):
        reads = [in0, in1]
        sc = scalar
        if isinstance(scalar, T):
            reads.append(scalar)
            sc = scalar.ap
        return self.op('dve', lambda e: e.scalar_tensor_tensor(out.ap, in0.ap, sc, in1.ap, op0, op1), reads, [out])

    def copy(self, eng, out, in_):
        if eng == 'act':
            return self.op('act', lambda e: e.copy(out.ap, in_.ap), [in_], [out])
        return self.op(eng, lambda e: e.tensor_copy(out.ap, in_.ap), [in_], [out])

    def memset(self, eng, out, val):
        return self.op(eng, lambda e: e.memset(out.ap, val), [], [out])

    def dma(self, q, out, in_, sem, reads=(), writes=()):
        oa = out.ap if isinstance(out, T) else out
        ia = in_.ap if isinstance(in_, T) else in_
        rd = list(reads) + ([in_] if isinstance(in_, T) else [])
        wr = list(writes) + ([out] if isinstance(out, T) else [])
        return self.op(q, lambda e: e.dma_start(out=oa, in_=ia), rd, wr, dsem=sem)

    def waitfor(self, eng, ops):
        if self.dry:
            return
        o = Op(eng, None, None)
        o.deps = [d for d in ops if d is not None]
        self.ops[eng].append(o)
        self.all.append(o)

    def emit(self):
        nc = self.nc
        for o in self.all:
            for d in o.deps:
                d.sig = True
        cnt = {}
        for o in self.all:
            if o.isdma:
                k = id(o.sem)
                cnt[k] = cnt.get(k, 0) + 16
                o.val = cnt[k]
        for e in ENG:
            c = 0
            for o in self.ops[e]:
                if o.isdma or o.fn is None:
                    continue
                if o.sig:
                    c += 1
                    o.val = c
                    o.sem = self.esem[e]
        with nc.Block() as blk:
            for e in ENG:
                def body(engine, e=e):
                    waited = {}
                    for o in self.ops[e]:
                        for d in o.deps:
                            k = id(d.sem)
                            if waited.get(k, 0) < d.val:
                                engine.wait_ge(d.sem, d.val)
                                waited[k] = d.val
                        if o.fn is None:
                            continue
                        ins = o.fn(engine)
                        if o.isdma:
                            ins.then_inc(o.sem, 16)
                        elif o.sig:
                            ins.then_inc(o.sem, 1)
                getattr(blk, BLKNAME[e])(body)


def vec_layout():
    lay = {}
    pos = [0]

    def add(name, w):
        lay[name] = (pos[0], w)
        pos[0] += w
    add('c', 8)
    for l in range(DEPTH):
        add('adab%d' % l, 72)
        add('ng%d' % l, 24)
    for j in range(2):
        add('gq%d' % j, 2)
        add('gk%d' % j, 1)
        add('subg%d' % j, 1)
        add('lam%d' % j, 4)
        add('og%d' % j, 8)
        add('lbl%d' % j, 8)
    add('b31', 8)
    return lay, pos[0]


def t5_bucket(d):
    d = np.maximum(d, 0)
    df = np.maximum(d, 1).astype(np.float32)
    large = 16 + (np.log(df / 16) / math.log(8) * 16).astype(np.int32)
    large = np.minimum(large, 31)
    return np.where(d < 16, d, large)


def fm(v):
    return np.ascontiguousarray(v.reshape(-1, 128).T)


def build(layers, stop_after=None):
    nc = bass.Bass("TRN2", target_bir_lowering=False)
    R = Rec(nc)
    lay, NV = vec_layout()

    def din(name, shape, dt=F32):
        return nc.dram_tensor(name, list(shape), dt, kind="ExternalInput").ap()

    xin = din("xin", [D, S])
    outT = nc.dram_tensor("outT", [D, S], F32, kind="ExternalOutput").ap()
    ada_w = din("ada_w", [DEPTH, 12, 128, 3, 2048])
    w_in = din("ffn_w_in", [DEPTH, 2, 11, 128, 2, 2048])
    w_down = din("ffn_w_down", [DEPTH, 2, 11, 128, 2, 1024])
    w_qkv = din("attn_w_qkv", [2, 8, 128, 2, 1536])
    w_ao = din("attn_w_o", [2, D, D])
    w_hin = din("hgrn_w_in", [2, 8, 128, 2, 2048])
    w_ho = din("hgrn_w_o", [2, D, D])
    vecs_d = din("vecs", [128, NV])
    biasg_d = din("biasg", [8, 128, 256])
    cmask_d = din("cmask", [128, 256])
    reset_d = din("resetm", [128, 512], BF16)
    reset16_d = din("reset16", [128, 512], BF16)
    caus_d = din("caus64", [64, 64])
    ident_d = din("ident", [128, 128])

    def sb(name, shape, dt):
        return nc.alloc_sbuf_tensor(name, list(shape), dt)

    xT_t = sb("xT", [128, 8, S], F32)
    hT_t = sb("hT", [128, 8, S], BF16)
    X = [[T(xT_t[:, c, n * TB:(n + 1) * TB]) for n in range(NB)] for c in range(8)]
    HT = [[T(hT_t[:, c, n * TB:(n + 1) * TB]) for n in range(NB)] for c in range(8)]
    ring_t = [sb("ring%d" % i, [128, SLOT], BF16) for i in range(RING)]
    ringP = [[T(ring_t[i][:, p * 1024:(p + 1) * 1024]) for p in range(6)] for i in range(RING)]
    semP = [[R.newsem() for p in range(6)] for _ in range(RING)]

    def RW(s, view):
        return ringP[s][0].v(view), ringP[s][1:6]
    SC = [T(sb("sc%d" % i, [128, 512], F32)[:, :]) for i in range(NSC)]
    BG_t = [sb("bg%d" % i, [128, S], BF16) for i in range(4)]
    QN = [T(BG_t[0][:, n * TB:(n + 1) * TB]) for n in range(NB)]
    QN1 = [T(BG_t[3][:, n * TB:(n + 1) * TB]) for n in range(NB)]
    KN = [T(BG_t[1][:, n * TB:(n + 1) * TB]) for n in range(NB)]
    VT = [T(BG_t[2][:, n * TB:(n + 1) * TB]) for n in range(NB)]
    vecs = T(sb("vecs_s", [128, NV], F32)[:, :])
    resetm = T(sb("resetm_s", [128, 512], BF16)[:, :])
    reset16 = T(sb("reset16_s", [128, 512], BF16)[:, :])
    cmask = T(sb("cmask_s", [128, 256], F32)[:, :])
    caus = T(sb("caus_s", [64, 64], F32)[:, :])
    ident = T(sb("ident_s", [128, 128], F32)[:, :])
    ones_bf = T(sb("ones_bf", [128, 128], BF16)[:, :])
    ones_f = T(sb("ones_f", [128, 128], F32)[:, :])
    bd_bf = T(sb("bd_bf", [128, 128], BF16)[:, :])
    biasT = [T(sb("biasT%d" % i, [128, 256], F32)[:, :]) for i in range(1)]
    BTt = [T(sb("BT%d" % i, [128, 256], F32)[:, :]) for i in range(1)]
    modT2 = [T(sb("modT%d" % i, [128, 72], F32)[:, :]) for i in range(2)]
    gs2 = [T(sb("gs%d" % i, [128, 24], F32)[:, :]) for i in range(2)]
    gt2 = [T(sb("gt%d" % i, [128, 24], F32)[:, :]) for i in range(2)]
    cact = T(sb("cact", [128, 8], BF16)[:, :])
    lbv = T(sb("lbv", [128, 8], F32)[:, :])
    oml = T(sb("oml", [128, 8], F32)[:, :])
    neglam = T(sb("neglam", [128, 4], F32)[:, :])
    lamt = T(sb("lamt", [128, 4], F32)[:, :])
    Sst = T(sb("Sst", [128, 128], F32)[:, :])
    Sbf2 = [T(sb("Sbf%d" % i, [128, 128], BF16)[:, :]) for i in range(2)]
    PS = [T(nc.alloc_psum_tensor("ps%d" % i, [128, 512], F32)[:, :]) for i in range(8)]

    def V(name, lo=0, w=None):
        s0, ww = lay[name]
        if w is None:
            w = ww - lo
        return vecs[:, s0 + lo:s0 + lo + w]

    class WRing:
        def __init__(self):
            self.jobs = []
            self.pos = 0
            self.issued = 0

        def _issue(self, k):
            s = k % RING
            for p0, p1, src, dstfn in self.jobs[k]:
                R.dma('pool', ringP[s][p0].v(dstfn(ring_t[s])), src, semP[s][p0], writes=ringP[s][p0 + 1:p1])

        def next(self, spec):
            if R.dry:
                self.jobs.append(spec)
                return 0
            k = self.pos
            self.pos += 1
            while self.issued < min(k + RING - 1, len(self.jobs)):
                self._issue(self.issued)
                self.issued += 1
            return k % RING

    W = WRing()

    def setup():
        sx = [R.newsem() for _ in range(8)]
        for c in range(8):
            R.dma('sp', T(xT_t[:, c, :]), xin[c * 128:(c + 1) * 128, :], sx[c], writes=X[c])
        for dst, src in ((vecs, vecs_d), (resetm, reset_d), (reset16, reset16_d), (cmask, cmask_d), (caus, caus_d), (ident, ident_d)):
            R.dma('sp', dst, src, R.newsem())
        R.memset('dve', ones_bf, 1.0)
        R.memset('dve', ones_f, 1.0)
        R.memset('dve', bd_bf, 0.0)
        R.memset('dve', bd_bf[0:64, 0:64], 1.0)
        R.memset('dve', bd_bf[64:128, 64:128], 1.0)
        R.act(cact, V('c'), AF.Silu)

    def ada_tasks(l):
        modT, gs, gt = modT2[l % 2], gs2[l % 2], gt2[l % 2]
        tasks = []
        for q in range(12):
            def task(q=q):
                src = ada_w[l, q]
                s = W.next([(0, 6, src, lambda rt: rt[:, :].rearrange("p (a n) -> p a n", a=3))])
                if R.dry:
                    return
                wv = ring_t[s][:, :].rearrange("p (kc n) -> p kc n", kc=8)
                for cc in range(6):
                    col = q * 6 + cc
                    for kc in range(8):
                        lt, er = RW(s, wv[:, kc, cc * 128:(cc + 1) * 128])
                        R.mm(PS[7][:, col:col + 1], lt, cact[:, kc:kc + 1], start=(kc == 0), stop=(kc == 7),
                             extra_reads=er)
            tasks.append(task)

        def fin():
            R.tt('dve', modT, PS[7][:, 0:72], V('adab%d' % l), ALU.add)
            for s in range(3):
                R.stt(gs[:, s * 8:(s + 1) * 8], modT[:, s * 24 + 8:s * 24 + 16], 1.0, V('ng%d' % l, s * 8, 8),
                      ALU.add, ALU.mult)
                R.ts('dve', gt[:, s * 8:(s + 1) * 8], modT[:, s * 24 + 16:s * 24 + 24], 0.5 if s != 1 else 1.0, None,
                     ALU.mult)
        tasks.append(fin)
        return tasks

    def norm(l, s):
        modT, gs = modT2[l % 2], gs2[l % 2]
        sq_eng = ['dve', 'pool', 'dve', 'pool', 'dve', 'pool', 'dve', 'dve']
        for n in range(NB):
            for c in range(8):
                sq = SC[c % 2].v(SC[c % 2].ap.bitcast(BF16)[:, (n % 2) * 512:(n % 2 + 1) * 512])
                R.tt(sq_eng[c], sq, X[c][n], X[c][n], ALU.mult)
                R.mm(PS[6], ones_bf, sq, start=(c == 0), stop=(c == 7))
            lnv = SC[2]
            rstd = SC[3 + n % 2]
            R.act(lnv, PS[6], AF.Ln, bias=EPS, scale=1.0 / D)
            R.act(rstd, lnv, AF.Exp, scale=-0.5)
            for c in range(8):
                tmp = SC[5 + c % 2]
                R.tt('dve', tmp, X[c][n], rstd, ALU.mult)
                R.act(HT[c][n], tmp, AF.Identity, bias=modT[:, s * 24 + c:s * 24 + c + 1],
                      scale=gs[:, s * 8 + c:s * 8 + c + 1])

    def ffn(l, s, inject=None):
        gt = gt2[l % 2]
        inject = inject or []
        fi = 0 if s == 0 else 1
        pending = []
        step = 0
        for g in range(11):
            sl = W.next([(0, 4, w_in[l, fi, g], lambda rt: rt[:, 0:4096].rearrange("p (a n) -> p a n", a=2)),
                         (4, 6, w_down[l, fi, g], lambda rt: rt[:, 4096:SLOT].rearrange("p (m d) -> p m d", m=2))])
            if R.dry:
                if inject:
                    inject.pop(0)()
                continue
            wab = ring_t[sl][:, 0:4096].rearrange("p (kc ab f) -> p kc ab f", kc=8, ab=2)
            wa0 = wab[:, :, 0, :]
            wa1 = wab[:, :, 1, :]
            wb = ring_t[sl][:, 4096:SLOT].rearrange("p (m d) -> p m d", m=2)
            for n in range(NB):
                us = []
                for m in range(2):
                    pa = PS[(step % 2) * 2]
                    pb = PS[(step % 2) * 2 + 1]
                    for kc in range(8):
                        lt, er = RW(sl, wa0[:, kc, m * 128:(m + 1) * 128])
                        R.mm(pa, lt, HT[kc][n], start=(kc == 0), stop=(kc == 7), extra_reads=er)
                    for kc in range(8):
                        lt, er = RW(sl, wa1[:, kc, m * 128:(m + 1) * 128])
                        R.mm(pb, lt, HT[kc][n], start=(kc == 0), stop=(kc == 7), extra_reads=er)
                    silt = SC[7 + step % 2]
                    bt = SC[9 + step % 2]
                    R.act(silt, pa, AF.Silu)
                    R.copy('act', bt, pb)
                    ut = SC[11 + (step % 4)]
                    u = ut.v(ut.ap.bitcast(BF16)[:, 0:512])
                    R.tt('pool', u, silt, bt, ALU.mult)
                    us.append(u)
                    step += 1
                    if len(pending) > (1 if m == 0 else 0) and pending:
                        pending.pop(0)()

                def mk_down(j0, us=us, n=n, sl=sl, wb=wb):
                    def down():
                        for j in range(j0, j0 + 4):
                            py = PS[4 + j % 3]
                            for m in range(2):
                                lt, er = RW(sl, wb[:, m, j * 128:(j + 1) * 128])
                                R.mm(py, lt, us[m], start=(m == 0), stop=(m == 1), extra_reads=er)
                            R.stt(X[j][n], py, gt[:, s * 8 + j:s * 8 + j + 1], X[j][n], ALU.mult, ALU.add)
                    return down
                pending.append(mk_down(0))
                pending.append(mk_down(4))
            if inject:
                inject.pop(0)()
                while pending:
                    pending.pop(0)()
        while pending:
            pending.pop(0)()
        while inject:
            inject.pop(0)()

    def attn(l):
        gt = gt2[l % 2]
        j = l // 2
        lam_init = 0.8 - 0.6 * math.exp(-0.3 * l)
        R.tt('dve', lamt[:, 0:1], V('lam%d' % j, 0, 1), V('lam%d' % j, 1, 1), ALU.mult)
        R.tt('dve', lamt[:, 1:2], V('lam%d' % j, 2, 1), V('lam%d' % j, 3, 1), ALU.mult)
        R.mm(PS[7][:, 0:2], ones_f, lamt[:, 0:2])
        R.act(lamt[:, 2:4], PS[7][:, 0:2], AF.Exp)
        R.tt('dve', neglam[:, 0:1], lamt[:, 3:4], lamt[:, 2:3], ALU.subtract)
        R.ts('dve', neglam[:, 1:2], neglam[:, 0:1], -lam_init, None, ALU.add)
        NL = neglam[:, 1:2]
        st = {'tix': 0, 'pidx': 0}
        epi = []
        for h in range(8):
            srcB = w_ao[j][h * 128:(h + 1) * 128, :]
            spec = [(0, 3, w_qkv[j, h], lambda rt: rt[:, 0:3072].rearrange("p (a n) -> p a n", a=2)),
                    (3, 4, srcB, lambda rt: rt[:, 3072:4096])]
            sl = W.next(spec)
            if R.dry:
                continue
            wa = ring_t[sl][:, 0:3072].rearrange("p (kc t f) -> p kc t f", kc=8, t=3)
            wo = ring_t[sl][:, 3072:4096]
            bT = biasT[0]
            BT = BTt[0]
            R.dma('sp', bT, biasg_d[h], R.newsem())
            R.tt('pool', BT, bT, cmask, ALU.add)
            b31 = V('b31', h, 1)
            tails = []
            kidx = 0
            for which, dstl, gain, lnb in ((0, QN, V('gq%d' % j, 0, 1), math.log(0.125)), (1, KN, V('gk%d' % j), 0.0)):
                for n in range(NB):
                    pq = PS[kidx % 2]
                    for kc in range(8):
                        lt, er = RW(sl, wa[:, kc, which, :])
                        R.mm(pq, lt, HT[kc][n], start=(kc == 0), stop=(kc == 7), extra_reads=er)
                    qs = SC[kidx % 2]
                    R.copy('act', qs, pq)
                    sq = SC[2].v(SC[2].ap.bitcast(BF16)[:, (kidx % 2) * 512:(kidx % 2 + 1) * 512])
                    R.tt('dve', sq, qs, qs, ALU.mult)

                    def tail(which=which, dstl=dstl, gain=gain, lnb=lnb, n=n, qs=qs, sq=sq, k2=kidx % 2):
                        R.mm(PS[2 + k2], bd_bf, sq)
                        lnv = SC[3].v(SC[3].ap[:, :]) if k2 == 0 else SC[9].v(SC[9].ap[:, :])
                        rs = SC[4].v(SC[4].ap[:, :]) if k2 == 0 else SC[10].v(SC[10].ap[:, :])
                        R.act(lnv, PS[2 + k2], AF.Ln, bias=EPS, scale=1.0 / 64)
                        R.act(rs, lnv, AF.Exp, bias=lnb, scale=-0.5)
                        R.stt(dstl[n], qs, gain, rs, ALU.mult, ALU.mult)
                        if which == 0:
                            R.stt(QN1[n], qs, V('gq%d' % j, 1, 1), rs, ALU.mult, ALU.mult)
                    tails.append(tail)
                    if len(tails) > 1:
                        tails.pop(0)()
                    kidx += 1
                    if epi:
                        epi.pop(0)()
            for tt_ in range(16):
                pv = PS[4 + (tt_ // 4) % 2]
                for kc in range(8):
                    lt, er = RW(sl, wa[:, kc, 2, :])
                    R.mm(pv[:, (tt_ % 4) * 128:(tt_ % 4 + 1) * 128],
                         HT[kc][tt_ // 4].v(hT_t[:, kc, tt_ * 128:(tt_ + 1) * 128]),
                         lt, start=(kc == 0), stop=(kc == 7), extra_reads=er)
                if tt_ % 4 == 3:
                    R.copy('dve', VT[tt_ // 4], pv)
                    while tails:
                        tails.pop(0)()
            for jq in range(NB):
                nk = 4 * jq + 4
                pend = []
                nd = list(range(0, max(4 * jq - 1, 0)))
                dg = list(range(max(4 * jq - 1, 0), nk))
                order = []
                while nd or dg:
                    if nd:
                        order.append(nd.pop(0))
                    if dg and (order or not nd):
                        order.append(dg.pop(0))
                for oi, i in enumerate(order):
                    delta = 128 * i - 512 * jq
                    c0 = max(delta, 0)
                    diag = i >= 4 * jq - 1
                    for c in range(2):
                        ps = PS[st['tix'] % 3]
                        st['tix'] += 1
                        kt = KN[i // 4].v(BG_t[1][:, i * 128:(i + 1) * 128])
                        qsrc = (QN, QN1)[c][jq]
                        qt = qsrc.v(BG_t[0 if c == 0 else 3][:, jq * 512 + c0:(jq + 1) * 512])
                        R.mm(ps[:, c0:512], kt, qt)
                        pk = st['pidx'] % 6
                        ptile = SC[5 + pk % 3]
                        st['pidx'] += 1
                        pt = ptile.v(ptile.ap.bitcast(BF16)[:, (pk // 3) * 512:(pk // 3 + 1) * 512])
                        if not diag:
                            R.act(pt, ps, AF.Exp, bias=b31)
                        else:
                            cb1 = min(delta + 256, 512)
                            tmp = SC[9 + c]
                            R.tt('dve', tmp[:, c0:cb1], ps[:, c0:cb1], BT[:, c0 - delta:cb1 - delta], ALU.add)
                            R.act(pt[:, c0:cb1], tmp[:, c0:cb1], AF.Exp)
                            if cb1 < 512:
                                R.act(pt[:, cb1:512], ps[:, cb1:512], AF.Exp, bias=b31)

                        def av(i=i, c=c, pt=pt, c0=c0, first=(oi == 0), last=(oi == nk - 1)):
                            vt = VT[i // 4][:, (i % 4) * 128:(i % 4 + 1) * 128]
                            R.mm(PS[4 + c][:, c0:512], vt, pt[:, c0:512], start=first, stop=last)
                            R.mm(PS[6 + c][:, c0:512], ones_bf, pt[:, c0:512], start=first, stop=last)
                        pend.append(av)
                        if len(pend) > 2:
                            pend.pop(0)()
                        if epi and oi * 2 + c >= 3:
                            epi.pop(0)()
                while pend:
                    pend.pop(0)()
                r0, r1, t0, t1, ob = SC[11], SC[12], SC[13], SC[14], SC[15]
                R.op('dve', lambda e, a=r0, b=PS[6]: e.reciprocal(a.ap, b.ap), [PS[6]], [r0])
                R.copy('act', t0, PS[4])
                R.op('dve', lambda e, a=r1, b=PS[7]: e.reciprocal(a.ap, b.ap), [PS[7]], [r1])
                R.copy('act', t1, PS[5])
                R.tt('dve', t0, t0, r0, ALU.mult)
                R.tt('pool', t1, t1, r1, ALU.mult)
                R.stt(ob, t1, NL, t0, ALU.mult, ALU.add)
                sq = SC[8].v(SC[8].ap.bitcast(BF16)[:, 0:512])
                R.tt('pool', sq, ob, ob, ALU.mult)
                oh = SC[8].v(SC[8].ap.bitcast(BF16)[:, 512:1024])

                def stage_b(sq=sq, ob=ob, oh=oh):
                    R.mm(PS[3], ones_bf, sq)
                    R.act(SC[11], PS[3], AF.Ln, bias=EPS, scale=1.0 / 128)
                    R.act(SC[12], SC[11], AF.Exp, bias=math.log(1.0 - lam_init), scale=-0.5)
                    R.stt(oh, ob, V('subg%d' % j), SC[12], ALU.mult, ALU.mult)
                epi.append(stage_b)
                for jj in range(8):
                    def stage_c(jj=jj, jq=jq, oh=oh, sl=sl, wo=wo):
                        lt, er = RW(sl, wo[:, jj * 128:(jj + 1) * 128])
                        R.mm(PS[3], lt, oh, extra_reads=er)
                        R.stt(X[jj][jq], PS[3], gt[:, 8 + jj:8 + jj + 1], X[jj][jq], ALU.mult, ALU.add)
                    epi.append(stage_c)
        while epi:
            epi.pop(0)()

    def hgrn(l):
        gt = gt2[l % 2]
        j = l // 2
        if j == 0:
            R.memset('dve', lbv, 0.0)
        else:
            R.tt('dve', lbv, V('lbl1'), V('lbl0'), ALU.subtract)
            R.act(lbv, lbv, AF.Sigmoid)
        R.ts('dve', oml, lbv, -1.0, 1.0, ALU.mult, ALU.add)
        sig, fg, lg, G, G16, eG, kk, kh, gsig, gsilu = SC[0:10]
        eL = gsig
        qq = SC[10].v(SC[10].ap.bitcast(BF16))
        Qs, qh = qq[:, 0:512], qq[:, 512:1024]
        KA = SC[11].v(SC[11].ap.bitcast(BF16))
        KB = SC[14].v(SC[14].ap.bitcast(BF16))
        Kp = [KA[:, 0:512], KA[:, 512:1024], KB[:, 0:512], KB[:, 512:1024]]
        vT = SC[12].v(SC[12].ap.bitcast(BF16).rearrange("p (c d) -> p c d", c=8)[0:64])
        khT = SC[13].v(SC[13].ap.bitcast(BF16).rearrange("p (c d) -> p c d", c=8)[0:64])
        BGH = [T(BG_t[k // 2][:, :].bitcast(F32)[:, (k % 2) * 512:(k % 2 + 1) * 512]) for k in range(8)]
        E = [SC[16], SC[17], BGH[5], BGH[6]]
        gsilu2 = [gsilu, BGH[0]]
        osb, lnv, tt_o = BGH[1], BGH[3], BGH[4]
        sqo = BGH[2].v(BGH[2].ap.bitcast(BF16))
        sq, oh = sqo[:, 0:512], sqo[:, 512:1024]
        outq = []
        blk = [0]

        def popq(k=1):
            for _ in range(k):
                if outq:
                    outq.pop(0)()
        for e_ in E:
            R.memset('pool', e_, 0.0)
        R.memset('dve', PS[7], 0.0)
        for h in range(8):
            srcB = w_ho[j][h * 128:(h + 1) * 128, :]
            spec = [(0, 4, w_hin[j, h], lambda rt: rt[:, 0:4096].rearrange("p (a n) -> p a n", a=2)),
                    (4, 5, srcB, lambda rt: rt[:, 4096:5120])]
            sl = W.next(spec)
            if R.dry:
                continue
            wa = ring_t[sl][:, 0:4096].rearrange("p (kc t f) -> p kc t f", kc=8, t=4)
            wo = ring_t[sl][:, 4096:5120]
            R.memset('dve', Sst, 0.0)
            R.memset('pool', Sbf2[0], 0.0)
            og = V('og%d' % j, h, 1)
            for n in range(NB):
                gsl = gsilu2[blk[0] % 2]
                blk[0] += 1
                for kc in range(8):
                    lt, er = RW(sl, wa[:, kc, 1, :])
                    R.mm(PS[0], lt, HT[kc][n], start=(kc == 0), stop=(kc == 7), extra_reads=er)
                R.act(sig, PS[0], AF.Sigmoid)
                for kc in range(8):
                    lt, er = RW(sl, wa[:, kc, 3, :])
                    R.mm(PS[2], lt, HT[kc][n], start=(kc == 0), stop=(kc == 7), extra_reads=er)
                R.act(gsig, PS[2], AF.Sigmoid)
                R.tt('dve', gsl, PS[2], gsig, ALU.mult)
                popq()
                R.ts('dve', fg, sig, oml[:, h:h + 1], lbv[:, h:h + 1], ALU.mult, ALU.add)
                R.act(lg, fg, AF.Ln)
                R.ts('pool', kk, fg, -1.0, 1.0, ALU.mult, ALU.add)
                popq()
                R.op('dve', lambda e, o_=G, d1=lg: e.tensor_tensor_scan(o_.ap, resetm.ap, d1.ap, 0.0, ALU.mult, ALU.add),
                     [resetm, lg], [G])
                R.op('dve', lambda e, o_=G16, d1=lg: e.tensor_tensor_scan(o_.ap, reset16.ap, d1.ap, 0.0, ALU.mult, ALU.add),
                     [reset16, lg], [G16])
                R.act(eG, G, AF.Exp)
                R.act(G16, G16, AF.Exp)
                popq()
                Gv = G.ap.rearrange("p (c t) -> p c t", c=8)
                for i_ in range(4):
                    w_ = 16 * (i_ + 1)
                    Ev = E[i_].v(E[i_].ap.rearrange("p (c t) -> p c t", c=8)[:, :, 0:w_])
                    if i_ == 0:
                        R.act(Ev, G.v(Gv[:, :, 0:w_]), AF.Exp, scale=-1.0)
                    else:
                        R.tt('dve', Ev, G.v(Gv[:, :, 16 * i_ - 1:16 * i_].to_broadcast([128, 8, w_])),
                             G.v(Gv[:, :, 0:w_]), ALU.subtract)
                        R.act(Ev, Ev, AF.Exp)
                R.tt('dve', eL, G.v(Gv[:, :, 63:64].to_broadcast([128, 8, 64])), G.v(Gv), ALU.subtract)
                R.act(eL, eL, AF.Exp)
                popq()
                for kc in range(8):
                    lt, er = RW(sl, wa[:, kc, 0, :])
                    R.mm(PS[1], lt, HT[kc][n], start=(kc == 0), stop=(kc == 7), extra_reads=er)
                R.tt('dve', Qs, PS[1], G16, ALU.mult)
                R.tt('dve', qh, PS[1], eG, ALU.mult)
                for i_ in range(4):
                    R.tt('dve' if i_ < 2 else 'pool', Kp[i_], kk, E[i_], ALU.mult)
                R.tt('pool', kh, kk, eL, ALU.mult)
                popq()
                for ci in range(8):
                    pv = PS[3 + ci // 4]
                    for kc in range(8):
                        lt, er = RW(sl, wa[:, kc, 2, :])
                        R.mm(pv[0:64, (ci % 4) * 128:(ci % 4 + 1) * 128],
                             HT[kc][n].v(hT_t[:, kc, n * 512 + ci * 64:n * 512 + ci * 64 + 64]),
                             lt, start=(kc == 0), stop=(kc == 7), extra_reads=er)
                R.copy('act', vT[:, 0:4, :], PS[3].v(PS[3].ap[0:64, :].rearrange("p (c d) -> p c d", c=4)))
                R.copy('act', vT[:, 4:8, :], PS[4].v(PS[4].ap[0:64, :].rearrange("p (c d) -> p c d", c=4)))
                popq()
                for ci in range(8):
                    pt_ = PS[5 + ci // 4]
                    R.tr(pt_[0:64, (ci % 4) * 128:(ci % 4 + 1) * 128], kh[:, ci * 64:ci * 64 + 64], ident)
                R.copy('dve', khT[:, 0:4, :], PS[5].v(PS[5].ap[0:64, :].rearrange("p (c d) -> p c d", c=4)))
                R.copy('dve', khT[:, 4:8, :], PS[6].v(PS[6].ap[0:64, :].rearrange("p (c d) -> p c d", c=4)))
                popq()
                PSO = PS[2]
                AbfAll = SC[15].v(SC[15].ap.bitcast(BF16)[0:64, 512:1024])
                for ci in range(8):
                    cs = ci * 64
                    for i_ in range(4):
                        w_ = 16 * (i_ + 1)
                        R.mm(PS[7][0:w_, cs + 16 * i_:cs + 16 * i_ + 16], Kp[i_][:, cs:cs + w_],
                             Qs[:, cs + 16 * i_:cs + 16 * i_ + 16])
                R.tt('dve', AbfAll.v(AbfAll.ap.rearrange("p (c t) -> p c t", c=8)),
                     PS[7].v(PS[7].ap[0:64, :].rearrange("p (c t) -> p c t", c=8)),
                     caus.v(caus.ap.rearrange("p (o t) -> p o t", o=1).to_broadcast([64, 8, 64])), ALU.mult)
                popq()
                for ci in range(8):
                    pu = PS[ci // 4][:, (ci % 4) * 128:(ci % 4 + 1) * 128]
                    R.mm(pu, khT[:, ci, :], vT[:, ci, :])
                popq(2)
                for ci in range(8):
                    cs = ci * 64
                    pu = PS[ci // 4][:, (ci % 4) * 128:(ci % 4 + 1) * 128]
                    R.mm(PSO[:, cs:cs + 64], vT[:, ci, :], AbfAll[:, cs:cs + 64], start=True, stop=False)
                    R.mm(PSO[:, cs:cs + 64], Sbf2[ci % 2], qh[:, cs:cs + 64], start=False, stop=True)
                    R.stt(Sbf2[(ci + 1) % 2], Sst, eG[:, cs + 63:cs + 64], pu, ALU.mult, ALU.add)
                    R.stt(Sst, Sst, eG[:, cs + 63:cs + 64], pu, ALU.mult, ALU.add)
                R.copy('act', osb, PSO)

                def s0(gsl=gsl, og=og):
                    R.tt('pool', sq, osb, osb, ALU.mult)
                    R.mm(PS[6], ones_bf, sq)
                    R.act(lnv, PS[6], AF.Ln, bias=EPS, scale=1.0 / 128)
                    R.act(lnv, lnv, AF.Exp, scale=-0.5)
                    R.stt(tt_o, osb, og, lnv, ALU.mult, ALU.mult)
                    R.tt('pool', oh, tt_o, gsl, ALU.mult)
                outq.append(s0)
                for jj in range(8):
                    def sy(jj=jj, n=n, sl=sl, wo=wo):
                        lt, er = RW(sl, wo[:, jj * 128:(jj + 1) * 128])
                        R.mm(PS[6], lt, oh, extra_reads=er)
                        R.stt(X[jj][n], PS[6], gt[:, 8 + jj:8 + jj + 1], X[jj][n], ALU.mult, ALU.add)
                    outq.append(sy)
        popq(100)

    def finish():
        so = [R.newsem() for _ in range(8)]
        outs = []
        for c in range(8):
            outs.append(R.dma('sp', outT[c * 128:(c + 1) * 128, :], T(xT_t[:, c, :]), so[c], reads=X[c]))
        R.waitfor('sp', [o for o in R.all if o.isdma])

    def program():
        if not R.dry:
            setup()
        done = False
        for idx, l in enumerate(layers):
            if idx == 0:
                for t_ in ada_tasks(l):
                    t_()
            for s in range(3):
                if not R.dry:
                    norm(l, s)
                if s == 1:
                    if l % 2 == 0:
                        attn(l)
                    else:
                        hgrn(l)
                else:
                    inj = None
                    last = stop_after is not None and (l, s) == tuple(stop_after)
                    if s == 2 and idx + 1 < len(layers) and not last:
                        inj = ada_tasks(layers[idx + 1])
                    ffn(l, s, inj)
                if stop_after is not None and (l, s) == tuple(stop_after):
                    done = True
                    break
            if done:
                break
        if not R.dry:
            finish()

    R.dry = True
    program()
    R.dry = False
    program()
    R.emit()
    return nc


LAUNCH_GROUPS = [[0, 1, 2, 3]]
_NC_CACHE = {}


def _get_nc(layers, stop_after=None):
    key = (tuple(layers), stop_after)
    if key not in _NC_CACHE:
        _NC_CACHE[key] = build(list(layers), stop_after)
    return _NC_CACHE[key]


def host_consts(inp, b):
    lay, NV = vec_layout()
    vecs = np.zeros((128, NV), np.float32)

    def put(name, arr):
        s0, w = lay[name]
        vecs[:, s0:s0 + w] = arr.reshape(128, w)
    put('c', fm(inp['c'][b]))
    for l in range(DEPTH):
        put('adab%d' % l, fm(inp['ada_b'][l]))
        put('ng%d' % l, fm(inp['norm_g'][l].reshape(-1)))
    for j in range(2):
        gq = np.zeros((128, 2), np.float32)
        gq[0:64, 0] = inp['attn_q_gain'][j]
        gq[64:128, 1] = inp['attn_q_gain'][j]
        put('gq%d' % j, gq)
        put('gk%d' % j, np.tile(inp['attn_k_gain'][j], 2).reshape(128, 1))
        put('subg%d' % j, inp['attn_subln_gain'][j].reshape(128, 1))
        lam = np.zeros((128, 4), np.float32)
        lam[0:64, :] = inp['attn_lambda'][j].T
        put('lam%d' % j, lam)
        put('og%d' % j, fm(inp['hgrn_out_gain'][j]))
        put('lbl%d' % j, fm(inp['hgrn_lb_logits'][j]))
    put('b31', np.broadcast_to(inp['rel_bias'][31][None, :], (128, 8)).copy())
    return vecs


def shared_consts(inp):
    kk = np.arange(128)[:, None]
    e = np.arange(256)[None, :]
    dist = e - kk
    bidx = t5_bucket(dist)
    biasg = np.ascontiguousarray(np.transpose(inp['rel_bias'][bidx], (2, 0, 1))).astype(np.float32)
    cmask = np.where(dist >= 0, 0.0, -30000.0).astype(np.float32)
    resetm = np.ones((128, 512), np.float32)
    resetm[:, 0::64] = 0.0
    reset16 = np.ones((128, 512), np.float32)
    reset16[:, 0::16] = 0.0
    s_ = np.arange(64)[:, None]
    t_ = np.arange(64)[None, :]
    caus = (s_ <= t_).astype(np.float32)
    ident = np.eye(128, dtype=np.float32)
    import ml_dtypes
    return dict(biasg=biasg, cmask=cmask, resetm=resetm.astype(ml_dtypes.bfloat16), reset16=reset16.astype(ml_dtypes.bfloat16), caus64=caus, ident=ident)


WKEYS = ['ada_w', 'ffn_w_in', 'ffn_w_down', 'attn_w_qkv', 'attn_w_o', 'hgrn_w_in', 'hgrn_w_o']


def host_weights(inp):
    c = np.ascontiguousarray
    out = {}
    a = inp['ada_w'].reshape(DEPTH, 8, 128, 12, 768)
    out['ada_w'] = c(a.transpose(0, 3, 2, 1, 4)).reshape(DEPTH, 12, 128, 3, 2048)
    a = inp['ffn_w_in'].reshape(DEPTH, 2, 8, 128, 2, 11, 256)
    out['ffn_w_in'] = c(a.transpose(0, 1, 5, 3, 2, 4, 6)).reshape(DEPTH, 2, 11, 128, 2, 2048)
    a = inp['ffn_w_down'].reshape(DEPTH, 2, 11, 2, 128, 1024)
    out['ffn_w_down'] = c(a.transpose(0, 1, 2, 4, 3, 5))
    a = inp['attn_w_qkv'].reshape(2, 8, 128, 3, 8, 128)
    out['attn_w_qkv'] = c(a.transpose(0, 4, 2, 1, 3, 5)).reshape(2, 8, 128, 2, 1536)
    a = inp['hgrn_w_in'].reshape(2, 8, 128, 4, 8, 128)
    out['hgrn_w_in'] = c(a.transpose(0, 4, 2, 1, 3, 5)).reshape(2, 8, 128, 2, 2048)
    out['attn_w_o'] = inp['attn_w_o']
    out['hgrn_w_o'] = inp['hgrn_w_o']
    return out


def run_groups(inp, groups, cores=range(8), stop_after=None, xT0=None):
    cores = list(cores)
    inp = {k: np.ascontiguousarray(np.asarray(v, dtype=np.float32)) for k, v in inp.items()}
    sh = shared_consts(inp)
    base = host_weights(inp)
    base.update(sh)
    vec = [host_consts(inp, b) for b in cores]
    xT = [np.ascontiguousarray(inp['x'][b].T) for b in cores] if xT0 is None else xT0
    for layers in groups:
        nc = _get_nc(layers, stop_after)
        in_maps = []
        for i, b in enumerate(cores):
            m = dict(base)
            m['vecs'] = vec[i]
            m['xin'] = xT[i]
            in_maps.append(m)
        res = run_bass_kernel_spmd(nc, in_maps, core_ids=list(range(len(cores))))
        xT = [np.asarray(r['outT']) for r in res.results]
    return xT


def kernel(**inputs):
    xT = run_groups(inputs, LAUNCH_GROUPS)
    out = np.stack([np.ascontiguousarray(t.T) for t in xT], axis=0).astype(np.float32)
    return out
```
